# Optimizing a Trainium2 kernel written in Bass

```python
import math
import jax, jax.numpy as jnp
from jax import lax
import numpy as np

D_MODEL = 1024
BATCH = 4
SEQ = 4096
DEPTH = 2

N_MIXERS = 2
N_A_LAYERS = (DEPTH + 1) // 2
N_B_LAYERS = DEPTH // 2
HEAD_DIM = 64
ATTN_SCALE = HEAD_DIM ** -0.5
A_DILATED = ((128, 1), (512, 4), (2048, 16))
A_N_GROUPS = 3
A_HEADS_PER_GROUP = D_MODEL // HEAD_DIM
A_HEADS = A_N_GROUPS * A_HEADS_PER_GROUP
A_GROUP_WIDTH = A_HEADS_PER_GROUP * HEAD_DIM
A_IN_COLS = A_N_GROUPS * 3 * A_GROUP_WIDTH
B_HEADS = D_MODEL // HEAD_DIM
B_WIDTH = B_HEADS * HEAD_DIM
B_IN_COLS = 3 * B_WIDTH + B_HEADS
Q_BLOCK = 128
REL_BUCKETS = 32
REL_MAX_DIST = 2048
N_GROUPS = 4
EXPERTS_PER_GROUP = 4
N_EXPERTS = N_GROUPS * EXPERTS_PER_GROUP
EXPERT_FF = D_MODEL // 4
TOP_K_IN_GROUP = 2
EPS = 1e-6
NEG_INF = -1e30

kernel_name = "hybrid_dilated_fox_hmoe_adaln"


def rmsnorm(x, g):
    xf = x.astype(jnp.float32)
    y = xf * lax.rsqrt(jnp.mean(xf * xf, axis=-1, keepdims=True) + EPS)
    return (y * g.astype(jnp.float32)).astype(x.dtype)


def t5_bucket(dist):
    max_exact = REL_BUCKETS // 2
    d = jnp.maximum(dist, 0)
    large = max_exact + (jnp.log(jnp.maximum(d, 1).astype(jnp.float32) / max_exact)
                         / math.log(REL_MAX_DIST / max_exact)
                         * (REL_BUCKETS - max_exact)).astype(jnp.int32)
    large = jnp.minimum(large, REL_BUCKETS - 1)
    return jnp.where(d < max_exact, d, large)


def dilated_group_attention(q, k, v, bias_table, window, dilation):
    b, s, h, hd = q.shape
    r = dilation
    n = s // r
    n_pad = -(-n // Q_BLOCK) * Q_BLOCK
    nb = n_pad // Q_BLOCK
    w_sub = window // dilation

    def strided(t):
        t = t.reshape(b, n, r, h, hd).transpose(0, 2, 1, 3, 4).reshape(b * r, n, h, hd)
        t = jnp.pad(t, ((0, 0), (0, n_pad - n), (0, 0), (0, 0)))
        return t.reshape(b * r, nb, Q_BLOCK, h, hd)

    def with_prev(t):
        prev = jnp.pad(t[:, :-1], ((0, 0), (1, 0), (0, 0), (0, 0), (0, 0)))
        return jnp.concatenate([prev, t], axis=2)

    qs = strided(q)
    kc = with_prev(strided(k))
    vc = with_prev(strided(v))
    qi = jnp.arange(Q_BLOCK)[:, None]
    kj = jnp.arange(2 * Q_BLOCK)[None, :]
    dist = qi + Q_BLOCK - kj
    bias = bias_table[t5_bucket(dist * r)]
    blk = jnp.arange(nb)[:, None, None]
    valid = (dist >= 0) & (dist <= w_sub) & (blk * Q_BLOCK + qi - dist >= 0)
    logits = jnp.einsum('znqhd,znkhd->znhqk', qs, kc).astype(jnp.float32)
    logits = logits + bias.transpose(2, 0, 1).astype(jnp.float32)[None, None]
    logits = jnp.where(valid[None, :, None], logits, NEG_INF)
    m = jnp.max(logits, axis=-1, keepdims=True)
    p = jnp.exp(logits - m)
    denom = jnp.sum(p, axis=-1)
    out = jnp.einsum('znhqk,znkhd->znqhd', p, vc.astype(jnp.float32))
    out = out / denom.transpose(0, 1, 3, 2)[..., None]
    lse = m[..., 0] + jnp.log(denom)
    out = out.reshape(b, r, n_pad, h, hd)[:, :, :n].transpose(0, 2, 1, 3, 4).reshape(b, s, h, hd)
    lse = lse.transpose(0, 1, 3, 2).reshape(b, r, n_pad, h)[:, :, :n].transpose(0, 2, 1, 3).reshape(b, s, h)
    return out, lse


def dilated_mixer(h, w_in, w_out, q_gain, k_gain, rel_bias):
    b, s, _ = h.shape
    proj = (h @ w_in).reshape(b, s, A_N_GROUPS, 3, A_HEADS_PER_GROUP, HEAD_DIM)
    outs, lses = [], []
    for g, (window, dil) in enumerate(A_DILATED):
        q = rmsnorm(proj[:, :, g, 0], q_gain) * ATTN_SCALE
        k = rmsnorm(proj[:, :, g, 1], k_gain)
        v = proj[:, :, g, 2]
        tab = rel_bias[:, g * A_HEADS_PER_GROUP:(g + 1) * A_HEADS_PER_GROUP]
        o, l = dilated_group_attention(q, k, v, tab, window, dil)
        outs.append(o)
        lses.append(l)
    outs = jnp.stack(outs, axis=0)
    wts = jax.nn.softmax(jnp.stack(lses, axis=0), axis=0)
    o = jnp.sum(wts[..., None] * outs, axis=0).astype(h.dtype)
    return o.reshape(b, s, A_GROUP_WIDTH) @ w_out


def forgetting_mixer(h, w_in, f_bias, w_out, q_gain, k_gain):
    b, s, _ = h.shape
    proj = h @ w_in
    qkv = proj[..., :3 * B_WIDTH].reshape(b, s, 3, B_HEADS, HEAD_DIM)
    log_f = jax.nn.log_sigmoid((proj[..., 3 * B_WIDTH:] + f_bias).astype(jnp.float32))
    cum = jnp.cumsum(log_f, axis=1).transpose(0, 2, 1)
    q = rmsnorm(qkv[:, :, 0], q_gain) * ATTN_SCALE
    k = rmsnorm(qkv[:, :, 1], k_gain)
    v = qkv[:, :, 2].astype(jnp.float32)
    nb = s // Q_BLOCK
    qb = q.reshape(b, nb, Q_BLOCK, B_HEADS, HEAD_DIM).transpose(1, 0, 2, 3, 4)
    cb = cum.reshape(b, B_HEADS, nb, Q_BLOCK).transpose(2, 0, 1, 3)
    key_pos = jnp.arange(s)

    def block(args):
        i, q_i, c_i = args
        logits = jnp.einsum('bqhd,bkhd->bhqk', q_i, k).astype(jnp.float32)
        logits = logits + (c_i[..., None] - cum[:, :, None, :])
        q_pos = i * Q_BLOCK + jnp.arange(Q_BLOCK)
        mask = key_pos[None, :] <= q_pos[:, None]
        p = jax.nn.softmax(jnp.where(mask, logits, NEG_INF), axis=-1)
        return jnp.einsum('bhqk,bkhd->bqhd', p, v)

    o = lax.map(block, (jnp.arange(nb), qb, cb))
    o = o.transpose(1, 0, 2, 3, 4).reshape(b, s, B_WIDTH).astype(h.dtype)
    return o @ w_out


def hierarchical_moe(h, wg_r, bg_r, we_r, be_r, w_gate, w_up, w_down):
    b, s, d = h.shape
    t = h.reshape(-1, d)
    g_prob = jax.nn.softmax((t @ wg_r + bg_r).astype(jnp.float32), axis=-1)
    g_idx = jnp.argmax(g_prob, axis=-1)
    g_onehot = jax.nn.one_hot(g_idx, N_GROUPS, dtype=jnp.float32)
    g_w = jnp.max(g_prob, axis=-1, keepdims=True)
    e_logits = (t @ we_r + be_r).astype(jnp.float32).reshape(-1, N_GROUPS, EXPERTS_PER_GROUP)
    e_sel = jnp.einsum('nge,ng->ne', e_logits, g_onehot)
    top_v, top_i = lax.top_k(e_sel, TOP_K_IN_GROUP)
    top_w = jax.nn.softmax(top_v, axis=-1) * g_w
    in_group = jnp.sum(jax.nn.one_hot(top_i, EXPERTS_PER_GROUP, dtype=jnp.float32) * top_w[..., None], axis=1)
    gates = (g_onehot[:, :, None] * in_group[:, None, :]).reshape(-1, N_EXPERTS)
    hid = jax.nn.silu(jnp.einsum('nd,edf->nef', t, w_gate)) * jnp.einsum('nd,edf->nef', t, w_up)
    y = jnp.einsum('nef,efd->nd', hid * gates[..., None].astype(hid.dtype), w_down)
    return y.reshape(b, s, d)


def setup_inputs(seed: int = 0) -> dict:
    key = jax.random.key(seed)
    ks = jax.random.split(key, 24)
    nrm = jax.random.normal
    D = D_MODEL
    inp = {}
    inp["x"] = nrm(ks[0], (BATCH, SEQ, D), jnp.float32)
    inp["c"] = nrm(ks[1], (BATCH, D), jnp.float32)
    inp["w_ada"] = nrm(ks[2], (DEPTH, D, 6 * D), jnp.float32) * (0.5 * D ** -0.5)
    inp["b_ada"] = nrm(ks[3], (DEPTH, 6 * D), jnp.float32) * 0.02
    inp["norm_mix"] = 1.0 + 0.02 * nrm(ks[4], (DEPTH, D), jnp.float32)
    inp["norm_ffn"] = 1.0 + 0.02 * nrm(ks[5], (DEPTH, D), jnp.float32)
    inp["rel_bias"] = 0.2 * nrm(ks[6], (REL_BUCKETS, A_HEADS), jnp.float32)
    inp["a_w_in"] = nrm(ks[7], (N_A_LAYERS, D, A_IN_COLS), jnp.float32) * D ** -0.5
    inp["a_w_out"] = nrm(ks[8], (N_A_LAYERS, A_GROUP_WIDTH, D), jnp.float32) * A_GROUP_WIDTH ** -0.5
    inp["a_q_norm"] = 1.0 + 0.02 * nrm(ks[9], (N_A_LAYERS, HEAD_DIM), jnp.float32)
    inp["a_k_norm"] = 1.0 + 0.02 * nrm(ks[10], (N_A_LAYERS, HEAD_DIM), jnp.float32)
    inp["b_w_in"] = nrm(ks[11], (N_B_LAYERS, D, B_IN_COLS), jnp.float32) * D ** -0.5
    inp["b_f_bias"] = 3.0 + 0.1 * nrm(ks[12], (N_B_LAYERS, B_HEADS), jnp.float32)
    inp["b_w_out"] = nrm(ks[13], (N_B_LAYERS, B_WIDTH, D), jnp.float32) * B_WIDTH ** -0.5
    inp["b_q_norm"] = 1.0 + 0.02 * nrm(ks[14], (N_B_LAYERS, HEAD_DIM), jnp.float32)
    inp["b_k_norm"] = 1.0 + 0.02 * nrm(ks[15], (N_B_LAYERS, HEAD_DIM), jnp.float32)
    inp["router_group_w"] = nrm(ks[16], (DEPTH, D, N_GROUPS), jnp.float32) * D ** -0.5
    inp["router_group_b"] = 0.01 * nrm(ks[17], (DEPTH, N_GROUPS), jnp.float32)
    inp["router_expert_w"] = nrm(ks[18], (DEPTH, D, N_EXPERTS), jnp.float32) * D ** -0.5
    inp["router_expert_b"] = 0.01 * nrm(ks[19], (DEPTH, N_EXPERTS), jnp.float32)
    inp["w_gate"] = nrm(ks[20], (DEPTH, N_EXPERTS, D, EXPERT_FF), jnp.float32) * D ** -0.5
    inp["w_up"] = nrm(ks[21], (DEPTH, N_EXPERTS, D, EXPERT_FF), jnp.float32) * D ** -0.5
    inp["w_down"] = nrm(ks[22], (DEPTH, N_EXPERTS, EXPERT_FF, D), jnp.float32) * EXPERT_FF ** -0.5
    return inp


def reference(x, c, w_ada, b_ada, norm_mix, norm_ffn, rel_bias,
              a_w_in, a_w_out, a_q_norm, a_k_norm,
              b_w_in, b_f_bias, b_w_out, b_q_norm, b_k_norm,
              router_group_w, router_group_b, router_expert_w, router_expert_b,
              w_gate, w_up, w_down):
    b, s, d = x.shape
    ada_in = jax.nn.silu(c)
    for i in range(DEPTH):
        mod = (ada_in @ w_ada[i] + b_ada[i]).reshape(b, 6, d)
        sh_m, sc_m, g_m = mod[:, 0, None, :], mod[:, 1, None, :], mod[:, 2, None, :]
        sh_f, sc_f, g_f = mod[:, 3, None, :], mod[:, 4, None, :], mod[:, 5, None, :]
        hm = rmsnorm(x, norm_mix[i]) * (1 + sc_m) + sh_m
        j = i // N_MIXERS
        if i % N_MIXERS == 0:
            y = dilated_mixer(hm, a_w_in[j], a_w_out[j], a_q_norm[j], a_k_norm[j], rel_bias)
        else:
            y = forgetting_mixer(hm, b_w_in[j], b_f_bias[j], b_w_out[j], b_q_norm[j], b_k_norm[j])
        x = x + g_m * y
        hf = rmsnorm(x, norm_ffn[i]) * (1 + sc_f) + sh_f
        x = x + g_f * hierarchical_moe(hf, router_group_w[i], router_group_b[i],
                                       router_expert_w[i], router_expert_b[i],
                                       w_gate[i], w_up[i], w_down[i])
    return x
```

```python
import contextlib
import numpy as np
import concourse.bass as bass
import concourse.mybir as mybir
from concourse.bass_utils import run_bass_kernel_spmd

F32 = mybir.dt.float32
BF16 = mybir.dt.bfloat16
ALU = mybir.AluOpType
AF = mybir.ActivationFunctionType
AX = mybir.AxisListType

S = 4096
D = 1024
OWN = 2048
EPS = 1e-6
DIL = (1, 4, 16)
PAIRS = [[0, 1], [2, 3], [4, 5], [6, 7]]
COMPUTE = ("pe", "act", "dve", "pool")


class Buf:
    def __init__(self, name, excl=False):
        self.name = name
        self.writers = {}
        self.readers = {}
        self.excl = excl
        self.sem = {}


class Prog:
    def __init__(self, nc):
        self.nc = nc
        self.stack = contextlib.ExitStack()
        self.q = {e: [] for e in ("pe", "act", "dve", "pool", "sp")}
        self.sems = {}
        self.cnt = {}
        self.known = {e: {} for e in self.q}
        self.pending = {e: False for e in self.q}
        for e in COMPUTE:
            self.sems[e] = self.stack.enter_context(nc.semaphore("prog_" + e))
            self.cnt[e] = 0
        self.nsem = 0
        self.nbuf = 0
        self.dmasems = []
        self.free_sems = {}
        self.phase_bufs = []

    def buf(self, name=None, excl=False):
        self.nbuf += 1
        b = Buf(name or f"b{self.nbuf}", excl)
        if self.phase_bufs:
            self.phase_bufs[-1].append(b)
        return b

    def dma_sem(self, kind):
        fl = self.free_sems.setdefault(kind, [])
        if fl:
            return fl.pop()
        sem = self.new_sem("dma" + kind)
        self.cnt[sem] = 0
        self.dmasems.append(sem)
        return sem

    def begin_phase(self):
        self.phase_bufs.append([])

    def end_phase(self):
        for b in self.phase_bufs.pop():
            for kind, sem in b.sem.items():
                self.free_sems.setdefault(kind, []).append(sem)
            b.sem = {}

    def new_sem(self, name):
        self.nsem += 1
        return self.stack.enter_context(self.nc.semaphore(f"{name}_{self.nsem}"))

    def _deps(self, eng, reads, writes):
        deps = {}

        def need(key, val):
            if deps.get(key, 0) < val:
                deps[key] = val

        for b in reads:
            for k, v in b.writers.items():
                need(k, v)
            if b.excl:
                for k, v in b.readers.items():
                    need(k, v)
        for b in writes:
            for k, v in b.writers.items():
                need(k, v)
            for k, v in b.readers.items():
                need(k, v)
        out = []
        kn = self.known[eng]
        for k, v in deps.items():
            if k == eng and eng == "pe":
                continue
            if kn.get(k, 0) >= v:
                continue
            kn[k] = v
            out.append((k, v))
        return out

    def _semof(self, key):
        return self.sems[key] if isinstance(key, str) else key

    def op(self, eng, fn, reads=(), writes=(), inc=True):
        waits = [(self._semof(k), v) for k, v in self._deps(eng, reads, writes)]
        if inc:
            self.cnt[eng] += 1
            self.pending[eng] = False
            ticket = self.cnt[eng]
        else:
            ticket = self.cnt[eng] + 1
            self.pending[eng] = True
        sem = self.sems[eng]

        def emit(e, waits=waits, fn=fn, inc=inc, sem=sem):
            for s, v in waits:
                e.wait_ge(s, v)
            ins = fn(e)
            if inc:
                ins.then_inc(sem, 1)

        self.q[eng].append(emit)
        for b in reads:
            b.readers[eng] = ticket
        for b in writes:
            b.writers[eng] = ticket
        return ticket

    def dma(self, eng, out_ap, in_ap, reads=(), writes=(), **kw):
        assert len(writes) == 1
        wb = writes[0]
        kind = "sw" if eng == "pool" else "hw"
        if kind not in wb.sem:
            wb.sem[kind] = self.dma_sem(kind)
        waits = [(self._semof(k), v) for k, v in self._deps(eng, reads, writes)]
        sem = wb.sem[kind]
        self.cnt[sem] += 16
        ticket = self.cnt[sem]

        def emit(e, waits=waits, sem=sem, out_ap=out_ap, in_ap=in_ap, kw=kw):
            for s, v in waits:
                e.wait_ge(s, v)
            e.dma_start(out=out_ap, in_=in_ap, **kw).then_inc(sem, 16)

        self.q[eng].append(emit)
        for b in reads:
            b.readers[sem] = ticket
        wb.writers[sem] = ticket
        return ticket

    def collective(self, kind, in_t, out_t, reads, writes):
        eng = "pool"
        wb = writes[0]
        waits = [(self._semof(k), v) for k, v in self._deps(eng, reads, writes)]
        sem = self.new_sem("cc")
        self.cnt[sem] = 1

        def emit(e, waits=waits, sem=sem):
            for s, v in waits:
                e.wait_ge(s, v)
            e.collective_compute(kind, ALU.bypass, replica_groups=PAIRS,
                                 ins=[in_t.ap().opt()], outs=[out_t.ap().opt()]).then_inc(sem)
            e.wait_ge(sem, 1)

        self.q[eng].append(emit)
        for b in reads:
            b.readers[sem] = 1
        wb.writers[sem] = 1
        self.known[eng][sem] = 1

    def barrier(self):
        for eng in self.q:
            waits = []
            for k in list(COMPUTE) + self.dmasems:
                v = self.cnt[k]
                if v == 0 or k == eng:
                    continue
                if self.known[eng].get(k, 0) >= v:
                    continue
                self.known[eng][k] = v
                waits.append((self._semof(k), v))

            def emit(e, waits=waits):
                for s, v in waits:
                    e.wait_ge(s, v)

            self.q[eng].append(emit)

    def flush(self):
        nc = self.nc
        for eng in COMPUTE:
            if self.pending[eng]:
                raise RuntimeError(f"pending un-inc'd op on {eng}")
        q = self.q
        self.q = {e: [] for e in q}
        with nc.Block() as block:
            @block.tensor
            def _(e):
                for f in q["pe"]:
                    f(e)

            @block.scalar
            def _(e):
                for f in q["act"]:
                    f(e)

            @block.vector
            def _(e):
                for f in q["dve"]:
                    f(e)

            @block.gpsimd
            def _(e):
                for f in q["pool"]:
                    f(e)

            @block.sync
            def _(e):
                for f in q["sp"]:
                    f(e)


class Phase:
    def __init__(self, P, name):
        self.P = P
        self.nc = P.nc
        self.name = name
        self.stack = contextlib.ExitStack()
        self.n = 0

    def __enter__(self):
        self.stack.__enter__()
        self.P.begin_phase()
        return self

    def __exit__(self, *a):
        self.P.barrier()
        self.P.flush()
        self.P.end_phase()
        return self.stack.__exit__(*a)

    def sb(self, name, shape, dtype):
        self.n += 1
        t = self.stack.enter_context(self.nc.sbuf_tensor(f"{self.name}_{name}_{self.n}", list(shape), dtype))
        return t, self.P.buf(name)

    def ps(self, name, shape, dtype=F32):
        self.n += 1
        t = self.stack.enter_context(self.nc.psum_tensor(f"{self.name}_{name}_{self.n}", list(shape), dtype))
        return t, self.P.buf(name, excl=True)


def t5_onehot():
    oh = np.zeros((32, 3, 384), np.float32)
    for g, r in enumerate(DIL):
        for dist in range(0, 129):
            d = np.int32(dist * r)
            if d < 16:
                b = int(d)
            else:
                v = np.log(np.float32(max(d, 1)) / np.float32(16)) / np.float32(np.log(2048 / 16)) * np.float32(16)
                b = min(16 + int(np.float32(v)), 31)
            oh[b, g, 127 + dist] = 1.0
    return oh


def build(stop=None, nsteps=None, do2=True, donorm=True):
    nc = bass.Bass("TRN2", target_bir_lowering=False)

    def din(name, shape, dt=F32):
        return nc.dram_tensor(name, list(shape), dt, kind="ExternalInput").ap()

    def dout(name, shape, dt=F32):
        return nc.dram_tensor(name, list(shape), dt, kind="ExternalOutput").ap()

    x_b = din("x_b", [S, D])
    x_own = din("x_own", [OWN, D])
    c_col = din("c_col", [128, 8])
    w_ada = din("w_ada", [2, D, 6 * D])
    b_ada = din("b_ada", [2, 6 * D])
    norm_mix = din("norm_mix", [2, D])
    norm_ffn = din("norm_ffn", [2, D])
    relb = din("relb", [32, 3, 8])
    ohc = din("ohc", [32, 3, 384])
    a_w_in = din("a_w_in", [D, 2, 3, 768])
    a_w_out = din("a_w_out", [D, D])
    a_gq = din("a_gq", [128, 1])
    a_gk = din("a_gk", [128, 1])
    b_w_in = din("b_w_in", [D, 1544])
    b_fb = din("b_fb", [1, 8])
    b_w_out = din("b_w_out", [D, D])
    b_gq = din("b_gq", [128, 1])
    b_gk = din("b_gk", [128, 1])
    r_w = din("r_w", [2, D, 20])
    r_b = din("r_b", [2, 20])
    small = stop in ("M", "A1", "A", "A2", "X1")
    w_gate = din("w_gate", [2, 16, D, 256] if not small else [1, 1, 8, 8])
    w_up = din("w_up", [2, 16, D, 256] if not small else [1, 1, 8, 8])
    w_down = din("w_down", [2, 16, 256, D] if not small else [1, 1, 8, 8])
    sel = din("sel", [128, 2])
    out = dout("out", [OWN, D])

    modrow = nc.dram_tensor("modrow", [2, 6 * D], F32)
    mscr = nc.dram_tensor("mscr", [3, 8, 128, 384], F32)
    sendA = [nc.dram_tensor(f"sendA{c}", [128, S], BF16) for c in range(4)]
    recvA = [nc.dram_tensor(f"recvA{c}", [256, S], BF16) for c in range(4)]
    dbg = {}
    if stop == "A":
        dbg["oA"] = dout("dbg_oA", [512, S], BF16)
    if stop == "M":
        dbg["mod"] = dout("dbg_mod", [2, 6 * D])
        dbg["mscr"] = dout("dbg_mscr", [3, 8, 128, 384])
    if stop == "A1":
        dbg["hmT"] = dout("dbg_hmT", [128, 8, S], BF16)
    if stop == "A2":
        dbg["qkn"] = dout("dbg_qkn", [128, 512], BF16)
        dbg["QT"] = dout("dbg_QT", [2, 128, 2, 128], BF16)
        dbg["KT"] = dout("dbg_KT", [3, 128, 2, 128], BF16)
        dbg["Vp"] = dout("dbg_Vp", [3, 128, 4, 128], BF16)
        dbg["PT"] = dout("dbg_PT", [128, 4, 256], BF16)
        dbg["acc"] = dout("dbg_acc", [128, 4, S], F32)
        dbg["oA"] = dout("dbg_oA", [512, S], BF16)

    P = Prog(nc)
    b_modrow = P.buf("modrow")
    b_mscr = P.buf("mscr")
    b_sendA = [P.buf(f"sendA{c}") for c in range(4)]
    b_recvA = [P.buf(f"recvA{c}") for c in range(4)]
    b_out = P.buf("out")

    with Phase(P, "M") as ph:
        ccol, b_ccol = ph.sb("ccol", [128, 8], F32)
        scol, b_scol = ph.sb("scol", [128, 8], F32)
        wst = [ph.sb("wst", [128, 8, 512], F32) for _ in range(4)]
        badat2 = [ph.sb("bada", [1, 512], F32) for _ in range(2)]
        rowt = [ph.sb("rowt", [1, 512], F32) for _ in range(2)]
        pM, b_pM = ph.ps("pM", [128, 512])
        P.dma("sp", ccol[:], c_col, writes=[b_ccol])
        P.op("act", lambda e: e.activation(scol[:], ccol[:], AF.Silu), reads=[b_ccol], writes=[b_scol])
        it = 0
        for l in range(2):
            for cg in range(12):
                w_t, b_w = wst[it % 4]
                r_t, b_r = rowt[it % 2]
                badat, b_badat = badat2[it % 2]
                P.dma("sp" if it % 2 == 0 else "pool", w_t[:],
                      w_ada[l, :, cg * 512:(cg + 1) * 512].rearrange("(kc p) n -> p kc n", p=128), writes=[b_w])
                P.dma("sp", badat[:], b_ada[l:l + 1, cg * 512:(cg + 1) * 512], writes=[b_badat])
                for kc in range(8):
                    P.op("pe", lambda e, kc=kc, w_t=w_t: e.matmul(pM[0:1, :], scol[:, kc:kc + 1], w_t[:, kc, :],
                                                                   start=(kc == 0), stop=(kc == 7)),
                         reads=[b_scol, b_w], writes=[b_pM], inc=(kc == 7))
                P.op("dve", lambda e, r_t=r_t, badat=badat: e.tensor_tensor(r_t[:], pM[0:1, :], badat[:], ALU.add),
                     reads=[b_pM, b_badat], writes=[b_r])
                P.dma("sp", modrow.ap()[l:l + 1, cg * 512:(cg + 1) * 512], r_t[:], reads=[b_r], writes=[b_modrow])
                it += 1
        relt, b_relt = ph.sb("relt", [32, 3, 8], F32)
        oht, b_oht = ph.sb("oht", [32, 3, 384], F32)
        bvec, b_bvec = ph.sb("bvec", [8, 3, 384], F32)
        P.dma("sp", relt[:], relb, writes=[b_relt])
        P.dma("sp", oht[:], ohc, writes=[b_oht])
        for g in range(3):
            P.op("pe", lambda e, g=g: e.matmul(pM[0:8, 0:384], relt[:, g, :], oht[:, g, :], start=True, stop=True),
                 reads=[b_relt, b_oht], writes=[b_pM])
            P.op("act", lambda e, g=g: e.activation(bvec[:, g, :], pM[0:8, 0:384], AF.Exp),
                 reads=[b_pM], writes=[b_bvec])
        P.op("dve", lambda e: e.memset(bvec[:, :, 0:127], 0.0), writes=[b_bvec])
        P.op("dve", lambda e: e.memset(bvec[:, :, 256:384], 0.0), writes=[b_bvec])
        for g in range(3):
            P.dma("sp", mscr.ap()[g], bvec[:, g, :].unsqueeze(1).to_broadcast([8, 128, 384]),
                  reads=[b_bvec], writes=[b_mscr])

    if stop == "M":
        P.dma("sp", dbg["mod"], modrow.ap(), reads=[b_modrow], writes=[b_out])
        P.dma("sp", dbg["mscr"], mscr.ap(), reads=[b_mscr], writes=[b_out])
        P.barrier()
        P.flush()
        P.stack.close()
        return nc

    with Phase(P, "A") as ph:
        hmT, _ = ph.sb("hmT", [128, 8, S], BF16)
        b_hmT = [P.buf(f"hmT{t}") for t in range(32)]
        acc, b_acc = ph.sb("acc", [128, 4, S], F32)
        wA = [ph.sb("wA", [128, 8, 768], BF16) for _ in range(1)]
        wstg = [ph.sb("wstg", [128, 768], F32) for _ in range(2)]
        Et, b_E = ph.sb("E", [128, 3, 4, 256], F32)
        xt = [ph.sb("xt", [128, D], F32) for _ in range(2)]
        tmod2 = [ph.sb("tmod", [128, D], F32) for _ in range(2)]
        tmod, b_tmod = tmod2[0]
        hmb2 = [ph.sb("hmb", [128, D], BF16) for _ in range(2)]
        hmb, b_hmb = hmb2[0]
        Abc, b_Abc = ph.sb("Abc", [128, D], F32)
        Sbc, b_Sbc = ph.sb("Sbc", [128, D], F32)
        ssx, b_ssx = ph.sb("ssx", [128, 32], F32)
        rsx, b_rsx = ph.sb("rsx", [128, 32], F32)
        ident, b_ident = ph.sb("ident", [128, 128], BF16)
        onesf, b_onesf = ph.sb("onesf", [128, 64], F32)
        gq, b_gq_ = ph.sb("gq", [128, 1], F32)
        gk, b_gk_ = ph.sb("gk", [128, 1], F32)
        sq, b_sq = ph.sb("sq", [128, 512], F32)
        rec, b_rec = sq, b_sq
        ssq, b_ssq = ph.sb("ssq", [128, 8], F32)
        rsq, b_rsq = ph.sb("rsq", [128, 8], F32)
        qkn2 = [ph.sb("qkn", [128, 512], BF16) for _ in range(2)]
        qkn, b_qkn = qkn2[0]
        QT = [ph.sb("QT", [128, 2, 128], BF16) for _ in range(2)]
        KT = [ph.sb("KT", [128, 2, 128], BF16) for _ in range(3)]
        Vp = [ph.sb("Vp", [128, 4, 128], BF16) for _ in range(4)]
        ex, b_ex = ph.sb("ex", [128, 4, 256], F32)
        PT, b_PT = ph.sb("PT", [128, 4, 256], BF16)
        oTc = [ph.sb("oTc", [64, 512], BF16) for _ in range(2)]
        pPs = [ph.ps("pP", [128, 1024]) for _ in range(2)]
        b_pP1s = [P.buf("pP1", excl=True) for _ in range(2)]
        pT, b_pT = ph.ps("pT", [128, 8, 128], BF16)
        pS, b_pS = ph.ps("pS", [128, 4, 256])
        pO, b_pO = ph.ps("pO", [128, 4, 128])
        pB, b_pB = pO[:].rearrange("p h q -> p (h q)"), b_pO

        P.op("pool", lambda e: e.memset(ident[:], 1.0), writes=[b_ident])
        P.op("pool", lambda e: e.affine_select(ident[:], ident[:], [[-1, 128]], ALU.is_equal, 0.0,
                                               base=0, channel_multiplier=1), reads=[b_ident], writes=[b_ident])
        P.op("pool", lambda e: e.memset(onesf[:], 1.0), writes=[b_onesf])
        for i in range(4):
            P.op("pool", lambda e, i=i: e.memset(Vp[i][0][:], 1.0), writes=[Vp[i][1]])
        P.op("dve", lambda e: e.memset(ssx[:], 0.0), writes=[b_ssx])
        if stop == "A2":
            for i in range(2):
                P.op("pool", lambda e, i=i: e.memset(QT[i][0][:], 0.0), writes=[QT[i][1]])
            for i in range(3):
                P.op("pool", lambda e, i=i: e.memset(KT[i][0][:], 0.0), writes=[KT[i][1]])
            P.op("pool", lambda e: e.memset(PT[:], 0.0), writes=[b_PT])
            P.op("pool", lambda e: e.memset(acc[:], 0.0), writes=[b_acc])
        P.dma("sp", gq[:], a_gq, writes=[b_gq_])
        P.dma("sp", gk[:], a_gk, writes=[b_gk_])
        mr = modrow.ap()
        P.dma("sp", Sbc[:], mr[0:1, 0:D].to_broadcast([128, D]), reads=[b_modrow], writes=[b_Sbc])
        P.dma("sp", Abc[:], mr[0:1, D:2 * D].to_broadcast([128, D]), reads=[b_modrow], writes=[b_Abc])
        P.dma("sp", tmod[:], norm_mix[0:1, :].to_broadcast([128, D]), writes=[b_tmod])
        P.op("dve", lambda e: e.scalar_tensor_tensor(Abc[:], Abc[:], 1.0, tmod[:], ALU.add, ALU.mult),
             reads=[b_Abc, b_tmod], writes=[b_Abc])

        def a1_pre(tt):
            x_t, b_x = xt[tt % 2]
            tm_t, b_tm = tmod2[tt % 2]
            hb_t, b_hb = hmb2[tt % 2]
            P.dma("sp", x_t[:], x_b[tt * 128:(tt + 1) * 128, :], writes=[b_x])
            P.op("act", lambda e, x_t=x_t, tt=tt: e.activation(ex[:].rearrange("p h w -> p (h w)"), x_t[:], AF.Square, accum_out=ssx[:, tt:tt + 1]),
                 reads=[b_x], writes=[b_ex, b_ssx])
            P.op("act", lambda e, tt=tt: e.activation(rsx[:, tt:tt + 1], ssx[:, tt:tt + 1], AF.Ln, bias=EPS, scale=1.0 / D),
                 reads=[b_ssx], writes=[b_rsx])
            P.op("act", lambda e, tt=tt: e.activation(rsx[:, tt:tt + 1], rsx[:, tt:tt + 1], AF.Exp, scale=-0.5), reads=[b_rsx], writes=[b_rsx])
            P.op("dve", lambda e, x_t=x_t, tt=tt, tm_t=tm_t: e.scalar_tensor_tensor(tm_t[:], x_t[:], rsx[:, tt:tt + 1], Abc[:], ALU.mult, ALU.mult),
                 reads=[b_x, b_rsx, b_Abc], writes=[b_tm])
            P.op("pool", lambda e, tm_t=tm_t, hb_t=hb_t: e.tensor_tensor(hb_t[:], tm_t[:], Sbc[:], ALU.add),
                 reads=[b_tm, b_Sbc], writes=[b_hb])

        def a1_pe(tt):
            hb_t, b_hb = hmb2[tt % 2]
            for kc in range(8):
                P.op("pe", lambda e, kc=kc, hb_t=hb_t: e.transpose(pT[:, kc, :], hb_t[:, kc * 128:(kc + 1) * 128], ident[:]),
                     reads=[b_hb, b_ident], writes=[b_pT], inc=(kc == 7))

        def a1_back(tt):
            P.op("act", lambda e, tt=tt: e.activation(hmT[:, :, tt * 128:(tt + 1) * 128], pT[:], AF.Copy),
                 reads=[b_pT], writes=[b_hmT[tt]])

        a1_pre(0)
        a1_pe(0)
        for tt in range(32):
            if tt + 1 < 32:
                a1_pre(tt + 1)
            a1_back(tt)
            if tt + 1 < 32:
                a1_pe(tt + 1)
        if stop == "A1":
            P.dma("sp", dbg["hmT"], hmT[:], reads=b_hmT, writes=[b_out])
        stg = 0
        wsel = 0
        for p in range(2 if stop != "A1" else 0):
            for g in range(3):
                for h in range(4):
                    src = bass.AP(mscr, ((g * 8 + 4 * p + h) * 128) * 384 + 127, [[383, 128], [1, 256]])
                    P.dma("sp", Et[:, g, (h % 2) * 2 + h // 2, :], src, reads=[b_mscr], writes=[b_E])
            for g in range(3):
                r = DIL[g]
                w_t, b_w = wA[0]
                wsel += 1
                for kc in range(8):
                    s_t, b_s = wstg[stg % 2]
                    stg += 1
                    P.dma("sp", s_t[:], a_w_in[kc * 128:(kc + 1) * 128, p, g, :], writes=[b_s])
                    P.op("pool", lambda e, s_t=s_t, w_t=w_t, kc=kc: e.tensor_copy(w_t[:, kc, :], s_t[:]),
                         reads=[b_s], writes=[b_w])
                nblk = S // r // 128
                steps = [(j, nb) for j in range(r) for nb in range(nblk)]
                if nsteps is not None:
                    steps = steps[:nsteps] if (p == 0 and g == 0) else []

                def s1a(si, j, nb, part=None, r=r, w_t=w_t, b_w=b_w):
                    start = 128 * nb * r + j
                    stop_ = start + 127 * r + 1
                    tl0, tl1 = start // 128, (stop_ - 1) // 128
                    hb = b_hmT[tl0:tl1 + 1]
                    qn_t, b_qn = qkn2[si % 2]
                    pP, b_pP = pPs[si % 2]
                    b_pP1 = b_pP1s[si % 2]
                    kcs = range(8) if part is None else (range(0, 4) if part == 0 else range(4, 8))
                    for kc in kcs:
                        lhs = hmT[:, kc, start:stop_:r]
                        P.op("pe", lambda e, lhs=lhs, kc=kc, pP=pP: e.matmul(pP[:, 0:512], lhs, w_t[:, kc, 0:512],
                                                                      start=(kc == 0), stop=(kc == 7)),
                             reads=hb + [b_w], writes=[b_pP], inc=False)
                        P.op("pe", lambda e, lhs=lhs, kc=kc, pP=pP: e.matmul(pP[:, 512:768], lhs, w_t[:, kc, 512:768],
                                                                      start=(kc == 0), stop=(kc == 7)),
                             reads=hb + [b_w], writes=[b_pP1], inc=(kc == 7))
                    if part == 0:
                        return
                    P.op("act", lambda e, pP=pP: e.activation(sq[:], pP[:, 0:512], AF.Square), reads=[b_pP], writes=[b_sq])
                    v_t, b_v = Vp[si % 4]
                    P.op("act", lambda e, v_t=v_t, pP=pP: e.activation(v_t[:, :, 0:64], pP[:, 512:768].rearrange("p (h d) -> p h d", d=64), AF.Copy),
                         reads=[b_pP1], writes=[b_v])
                    P.op("dve", lambda e: e.tensor_reduce(ssq[:], sq[:].rearrange("p (h d) -> p h d", d=64), AX.X, ALU.add),
                         reads=[b_sq], writes=[b_ssq])
                    P.op("act", lambda e: e.activation(rsq[:], ssq[:], AF.Ln, bias=EPS, scale=1.0 / 64),
                         reads=[b_ssq], writes=[b_rsq])
                    P.op("act", lambda e: e.activation(rsq[:], rsq[:], AF.Exp, scale=-0.5), reads=[b_rsq], writes=[b_rsq])
                    P.op("dve", lambda e, qn_t=qn_t, pP=pP: e.tensor_tensor(qn_t[:].rearrange("p (h d) -> p h d", d=64),
                                                          pP[:, 0:512].rearrange("p (h d) -> p h d", d=64),
                                                          rsq[:].unsqueeze(2).to_broadcast([128, 8, 64]), ALU.mult),
                         reads=[b_pP, b_rsq], writes=[b_qn])

                def s1b(si, j, nb):
                    qn_t, b_qn = qkn2[si % 2]
                    for c4 in range(4):
                        P.op("pe", lambda e, c4=c4, qn_t=qn_t: e.transpose(pT[:, c4, :], qn_t[:, c4 * 128:(c4 + 1) * 128], ident[:]),
                             reads=[b_qn, b_ident], writes=[b_pT], inc=(c4 == 3))
                    q_t, b_q = QT[si % 2]
                    k_t, b_k = KT[si % 3]
                    P.op("act", lambda e, q_t=q_t: e.activation(q_t[:], pT[:, 0:2, :], AF.Copy, scale=gq[:, 0:1]),
                         reads=[b_pT, b_gq_], writes=[b_q])
                    P.op("dve", lambda e, k_t=k_t: e.tensor_scalar(k_t[:], pT[:, 2:4, :], gk[:, 0:1], None, ALU.mult),
                         reads=[b_pT, b_gk_], writes=[b_k])

                def s2a(si, j, nb, r=r, g=g, p=p):
                    q_t, b_q = QT[si % 2]
                    k_t, b_k = KT[si % 3]
                    kp_t, b_kp = KT[(si - 1) % 3]
                    W = 128 if nb == 0 else 256
                    last = 3
                    for h in range(4):
                        pr, hp = h // 2, h % 2
                        lo, hi = 64 * hp, 64 * hp + 64
                        hq = hp * 2 + pr
                        P.op("pe", lambda e, hq=hq, pr=pr, lo=lo, hi=hi: e.matmul(pS[:, hq, 0:128], k_t[lo:hi, pr, :], q_t[lo:hi, pr, :],
                                                                               start=True, stop=True),
                             reads=[b_k, b_q], writes=[b_pS], inc=(nb == 0 and h == last))
                        if nb > 0:
                            P.op("pe", lambda e, hq=hq, pr=pr, lo=lo, hi=hi: e.matmul(pS[:, hq, 128:256], kp_t[lo:hi, pr, :], q_t[lo:hi, pr, :],
                                                                                   start=True, stop=True),
                                 reads=[b_kp, b_q], writes=[b_pS], inc=(h == last))
                    P.op("act", lambda e, W=W: e.activation(ex[:, :, 0:W], pS[:, :, 0:W], AF.Exp, scale=0.125),
                         reads=[b_pS], writes=[b_ex])
                    P.op("dve", lambda e, W=W, g=g: e.tensor_tensor(PT[:, :, 0:W], ex[:, :, 0:W], Et[:, g, :, 0:W], ALU.mult),
                         reads=[b_ex, b_E], writes=[b_PT])

                def s2b(si, j, nb, r=r, g=g, p=p):
                    start = 128 * nb * r + j
                    stop_ = start + 127 * r + 1
                    v_t, b_v = Vp[si % 4]
                    vp_t, b_vp = Vp[(si - 1) % 4]
                    last = 3
                    for h in range(4):
                        hq = (h % 2) * 2 + h // 2
                        P.op("pe", lambda e, h=h, hq=hq: e.matmul(pO[:, h, :], v_t[:, h, :], PT[:, hq, 0:128],
                                                           start=True, stop=(nb == 0)),
                             reads=[b_v, b_PT], writes=[b_pO], inc=(nb == 0 and h == last))
                        if nb > 0:
                            P.op("pe", lambda e, h=h, hq=hq: e.matmul(pO[:, h, :], vp_t[:, h, :], PT[:, hq, 128:256],
                                                               start=False, stop=True),
                                 reads=[b_vp, b_PT], writes=[b_pO], inc=(h == last))
                    av = acc[:, :, start:stop_:r]
                    if g == 0:
                        P.op("act", lambda e, av=av: e.activation(av, pO[:], AF.Copy), reads=[b_pO], writes=[b_acc])
                    else:
                        P.op("dve", lambda e, av=av: e.tensor_tensor(av, av, pO[:], ALU.add),
                             reads=[b_pO, b_acc], writes=[b_acc])

                n_ = len(steps)
                if n_ > 0:
                    s1a(0, *steps[0])
                if n_ > 1:
                    s1a(1, *steps[1])
                if n_ > 0:
                    s1b(0, *steps[0])
                for si in range(n_):
                    if do2:
                        s2a(si, *steps[si])
                    if si + 2 < n_:
                        s1a(si + 2, *steps[si + 2], part=0)
                    if si + 1 < n_:
                        s1b(si + 1, *steps[si + 1])
                    if si + 2 < n_:
                        s1a(si + 2, *steps[si + 2], part=1)
                    if do2:
                        s2b(si, *steps[si])

            k = 0
            recA = [(sq, b_sq), (ex[:].rearrange("p h w -> p (h w)"), b_ex)]
            pBA = [(pO[:].rearrange("p h q -> p (h q)"), b_pO), (pS[:].rearrange("p h w -> p (h w)"), b_pS)]
            for h in range(4 if donorm else 0):
                for tg in range(8):
                    ts_ = slice(tg * 512, (tg + 1) * 512)
                    o_t, b_o = oTc[k % 2]
                    rec, b_rec = recA[k % 2]
                    pB, b_pB = pBA[k % 2]
                    k += 1
                    P.op("act", lambda e, h=h, ts_=ts_, rec=rec: e.activation(rec[64:65, 0:512], acc[64:65, h, ts_], AF.Ln),
                         reads=[b_acc], writes=[b_rec])
                    P.op("act", lambda e, rec=rec: e.activation(rec[64:65, 0:512], rec[64:65, 0:512], AF.Exp, scale=-1.0),
                         reads=[b_rec], writes=[b_rec])
                    P.op("pe", lambda e, rec=rec, pB=pB: e.matmul(pB[0:64, 0:512], onesf[64:65, 0:64], rec[64:65, 0:512], start=True, stop=True),
                         reads=[b_onesf, b_rec], writes=[b_pB])
                    P.op("dve", lambda e, h=h, ts_=ts_, o_t=o_t, pB=pB: e.tensor_tensor(o_t[:], acc[0:64, h, ts_], pB[0:64, 0:512], ALU.mult),
                         reads=[b_acc, b_pB], writes=[b_o])
                    slot = 4 * p + h
                    row = (slot % 2) * 64
                    P.dma("sp", sendA[slot // 2].ap()[row:row + 64, ts_], o_t[:], reads=[b_o], writes=[b_sendA[slot // 2]])
        if stop == "A2":
            P.dma("sp", dbg["qkn"], qkn[:], reads=[b_qkn], writes=[b_out])
            for i in range(2):
                P.dma("sp", dbg["QT"][i], QT[i][0][:], reads=[QT[i][1]], writes=[b_out])
            for i in range(3):
                P.dma("sp", dbg["KT"][i], KT[i][0][:], reads=[KT[i][1]], writes=[b_out])
                P.dma("sp", dbg["Vp"][i], Vp[i][0][:], reads=[Vp[i][1]], writes=[b_out])
            P.dma("sp", dbg["PT"], PT[:], reads=[b_PT], writes=[b_out])
            P.dma("sp", dbg["acc"], acc[:], reads=[b_acc], writes=[b_out])

    if stop == "A1":
        P.stack.close()
        return nc
    if stop in ("A", "A2"):
        for c in range(4):
            P.dma("sp", dbg["oA"][c * 128:(c + 1) * 128, :], sendA[c].ap(), reads=[b_sendA[c]], writes=[b_out])
        P.barrier()
        P.flush()
        P.stack.close()
        return nc

    sendH = [nc.dram_tensor(f"sendH{c}", [256, OWN], BF16) for c in range(4)]
    recvH = [nc.dram_tensor(f"recvH{c}", [512, OWN], BF16) for c in range(4)]
    xmid = nc.dram_tensor("xmid", [OWN, D], F32)
    sendC = [nc.dram_tensor(f"sendC{c}", [128, S], BF16) for c in range(4)]
    recvC = [nc.dram_tensor(f"recvC{c}", [256, S], BF16) for c in range(4)]
    b_sendH = [P.buf(f"sendH{c}") for c in range(4)]; b_recvH = [P.buf(f"recvH{c}") for c in range(4)]; b_xmid = P.buf("xmid")
    b_sendC = [P.buf(f"sendC{c}") for c in range(4)]; b_recvC = [P.buf(f"recvC{c}") for c in range(4)]
    mr = modrow.ap()

    def phase_B(l, send_t, b_send, recv_t, b_recv, w_out_ap, stopB=None):
        with Phase(P, f"B{l}") as po:
            xres, _ = po.sb("xres", [128, 16, D], F32)
            b_xr = [P.buf(f"xres{t}") for t in range(16)]
            hfT, b_hfT = po.sb("hfT", [128, 8, OWN], BF16)
            gT, b_gT = po.sb("gT", [16, OWN], BF16)
            with Phase(P, f"B1{l}") as ph:
                selt, b_selt = ph.sb("selt", [128, 2], F32)
                Gbc, b_Gbc = ph.sb("Gbc", [128, D], F32)
                woB, b_woB = ph.sb("woB", [128, 8, D], BF16)
                wst = [ph.sb("wst", [128, D], F32) for _ in range(2)]
                cand = [[ph.sb("cand", [128, 8, 512], BF16) for _ in range(2)] for _ in range(2)]
                otmp, b_otmp = ph.sb("otmp", [128, 8, 512], BF16)
                osel = [ph.sb("osel", [128, 8, 512], BF16) for _ in range(2)]
                pY = [ph.ps("pY", [128, 1024]) for _ in range(2)]
                P.dma("sp", selt[:], sel, writes=[b_selt])
                P.dma("sp", Gbc[:], mr[l:l + 1, 2 * D:3 * D].to_broadcast([128, D]), reads=[b_modrow], writes=[b_Gbc])
                xsrc = x_own if l == 0 else xmid.ap()
                for t4 in range(4):
                    for t in range(4 * t4, 4 * t4 + 4):
                        P.dma("sp", xres[:, t, :], xsrc[t * 128:(t + 1) * 128, :],
                              reads=([b_xmid] if l == 1 else []), writes=[b_xr[t]])
                for kc in range(8):
                    w_t, b_w = wst[kc % 2]
                    P.dma("sp", w_t[:], w_out_ap[kc * 128:(kc + 1) * 128, :], writes=[b_w])
                    P.op("dve", lambda e, w_t=w_t, kc=kc: e.tensor_tensor(woB[:, kc, :], w_t[:], Gbc[:], ALU.mult),
                         reads=[b_w, b_Gbc], writes=[b_woB])
                for c in range(4):
                    P.collective("AllGather", send_t[c], recv_t[c], reads=[b_send[c]], writes=[b_recv[c]])
                for tg in range(4):
                    c0_, c1_ = cand[tg % 2]
                    o_t, b_o = osel[tg % 2]
                    for rk, (c_t, b_c) in enumerate((c0_, c1_)):
                        off = rk * OWN + tg * 512
                        for kc in range(8):
                            P.dma("sp", c_t[:, kc, :], recv_t[kc % 4].ap()[(kc // 4) * 128:(kc // 4 + 1) * 128, off:off + 512],
                                  reads=[b_recv[kc % 4]], writes=[b_c])
                    P.op("dve", lambda e, c_t=c0_[0]: e.tensor_scalar(otmp[:], c_t[:], selt[:, 0:1], None, ALU.mult),
                         reads=[c0_[1], b_selt], writes=[b_otmp])
                    P.op("dve", lambda e, c_t=c1_[0], o_t=o_t: e.scalar_tensor_tensor(o_t[:], c_t[:], selt[:, 1:2], otmp[:], ALU.mult, ALU.add),
                         reads=[c1_[1], b_selt, b_otmp], writes=[b_o])
                    for t4 in range(4):
                        T = tg * 4 + t4
                        y_t, b_y = pY[T % 2]
                        for half in range(2):
                            for kc in range(8):
                                P.op("pe", lambda e, o_t=o_t, kc=kc, t4=t4, half=half, y_t=y_t: e.matmul(
                                    y_t[:, half * 512:(half + 1) * 512], o_t[:, kc, t4 * 128:(t4 + 1) * 128],
                                    woB[:, kc, half * 512:(half + 1) * 512], start=(kc == 0), stop=(kc == 7)),
                                    reads=[b_o, b_woB], writes=[b_y], inc=(kc == 7 and half == 1))
                        P.op("dve", lambda e, T=T, y_t=y_t: e.tensor_tensor(xres[:, T, :], xres[:, T, :], y_t[:], ALU.add),
                             reads=[b_y, b_xr[T]], writes=[b_xr[T]])
            if stopB == "B1":
                P.dma("sp", dbg["x"], xres[:], reads=b_xr, writes=[b_out])
                return
            NS = 5
            Gf, b_Gf = po.sb("Gf", [128, D], F32)
            wg = [po.sb("wg", [128, 8, 256], BF16) for _ in range(NS)]
            wu = [po.sb("wu", [128, 8, 256], BF16) for _ in range(NS)]
            wd = [po.sb("wd", [128, 2, D], BF16) for _ in range(NS)]
            stg = [po.sb("stg", [128, 2, D], F32) for _ in range(1)]
            P.dma("sp", Gf[:], mr[l:l + 1, 5 * D:6 * D].to_broadcast([128, D]), reads=[b_modrow], writes=[b_Gf])
            sc_ = [0]

            def load_expert(e_):
                s_ = e_ % NS
                P.dma("pool", wg[s_][0][:], w_gate[l, e_].rearrange("(kc p) f -> p kc f", p=128), writes=[wg[s_][1]])
                P.dma("pool", wu[s_][0][:], w_up[l, e_].rearrange("(kc p) f -> p kc f", p=128), writes=[wu[s_][1]])
                st_t, b_st = stg[0]
                sc_[0] += 1
                P.dma("sp", st_t[:], w_down[l, e_].rearrange("(fc p) d -> p fc d", p=128), writes=[b_st])
                P.op("dve", lambda e, st_t=st_t, s_=s_: e.tensor_tensor(wd[s_][0][:], st_t[:], Gf[:].unsqueeze(1).to_broadcast([128, 2, D]), ALU.mult),
                     reads=[b_st, b_Gf], writes=[wd[s_][1]])

            with Phase(P, f"B2{l}") as ph:
                Abc, b_Abc = ph.sb("Abc", [128, D], F32)
                Sbc, b_Sbc = ph.sb("Sbc", [128, D], F32)
                tmod, b_tmod = ph.sb("tmod", [128, D], F32)
                junk, b_junk = ph.sb("junk", [128, D], BF16)
                hf32 = [ph.sb("hf32", [128, D], F32) for _ in range(1)]
                hfT32 = [ph.sb("hfT32", [128, 8, 128], F32) for _ in range(1)]
                identf, b_identf = ph.sb("identf", [128, 128], F32)
                identb, b_identb = ph.sb("identb", [128, 128], BF16)
                ssx, b_ssx = ph.sb("ssx", [128, 16], F32)
                rsx, b_rsx = ph.sb("rsx", [128, 16], F32)
                rw, b_rw = ph.sb("rw", [128, 8, 20], F32)
                rbb, b_rbb = ph.sb("rbb", [128, 20], F32)
                lg, b_lg = ph.sb("lg", [128, 16, 20], F32)
                pXf = [ph.ps("pXf", [128, 8, 128]) for _ in range(2)]
                pR, b_pR = ph.ps("pR", [128, 512])
                pGT, b_pGT = ph.ps("pGT", [128, 1024], BF16)
                P.op("pool", lambda e: e.memset(identf[:], 1.0), writes=[b_identf])
                P.op("pool", lambda e: e.affine_select(identf[:], identf[:], [[-1, 128]], ALU.is_equal, 0.0,
                                                       base=0, channel_multiplier=1), reads=[b_identf], writes=[b_identf])
                P.op("pool", lambda e: e.tensor_copy(identb[:], identf[:]), reads=[b_identf], writes=[b_identb])
                P.op("dve", lambda e: e.memset(ssx[:], 0.0), writes=[b_ssx])
                P.dma("sp", Sbc[:], mr[l:l + 1, 3 * D:4 * D].to_broadcast([128, D]), reads=[b_modrow], writes=[b_Sbc])
                P.dma("sp", Abc[:], mr[l:l + 1, 4 * D:5 * D].to_broadcast([128, D]), reads=[b_modrow], writes=[b_Abc])
                P.dma("sp", tmod[:], norm_ffn[l:l + 1, :].to_broadcast([128, D]), writes=[b_tmod])
                P.op("dve", lambda e: e.scalar_tensor_tensor(Abc[:], Abc[:], 1.0, tmod[:], ALU.add, ALU.mult),
                     reads=[b_Abc, b_tmod], writes=[b_Abc])
                P.dma("sp", rw[:], r_w[l].rearrange("(kc p) n -> p kc n", p=128), writes=[b_rw])
                P.dma("sp", rbb[:], r_b[l:l + 1, :].to_broadcast([128, 20]), writes=[b_rbb])
                def b2_pre(T):
                    h_t, b_h = hf32[0]
                    P.op("act", lambda e, T=T: e.activation(junk[:], xres[:, T, :], AF.Square, accum_out=ssx[:, T:T + 1]),
                         reads=[b_xr[T]], writes=[b_junk, b_ssx])
                    P.op("act", lambda e, T=T: e.activation(rsx[:, T:T + 1], ssx[:, T:T + 1], AF.Ln, bias=EPS, scale=1.0 / D),
                         reads=[b_ssx], writes=[b_rsx])
                    P.op("act", lambda e, T=T: e.activation(rsx[:, T:T + 1], rsx[:, T:T + 1], AF.Exp, scale=-0.5), reads=[b_rsx], writes=[b_rsx])
                    P.op("dve", lambda e, T=T: e.scalar_tensor_tensor(tmod[:], xres[:, T, :], rsx[:, T:T + 1], Abc[:], ALU.mult, ALU.mult),
                         reads=[b_xr[T], b_rsx, b_Abc], writes=[b_tmod])
                    P.op("pool", lambda e, h_t=h_t: e.tensor_tensor(h_t[:], tmod[:], Sbc[:], ALU.add),
                         reads=[b_tmod, b_Sbc], writes=[b_h])

                def b2_pe1(T):
                    h_t, b_h = hf32[0]
                    x_t, b_xT = pXf[T % 2]
                    for kc in range(8):
                        P.op("pe", lambda e, kc=kc, h_t=h_t, x_t=x_t: e.transpose(x_t[:, kc, :], h_t[:, kc * 128:(kc + 1) * 128], identf[:]),
                             reads=[b_h, b_identf], writes=[b_xT], inc=(kc == 7))

                def b2_back(T):
                    x_t, b_xT = pXf[T % 2]
                    f_t, b_f = hfT32[0]
                    P.op("act", lambda e, T=T, x_t=x_t: e.activation(hfT[:, :, T * 128:(T + 1) * 128], x_t[:], AF.Copy),
                         reads=[b_xT], writes=[b_hfT])
                    P.op("dve", lambda e, x_t=x_t, f_t=f_t: e.tensor_copy(f_t[:], x_t[:]), reads=[b_xT], writes=[b_f])
                    for kc in range(8):
                        P.op("pe", lambda e, kc=kc, f_t=f_t: e.matmul(pR[:, 0:20], f_t[:, kc, :], rw[:, kc, :], start=(kc == 0), stop=(kc == 7)),
                             reads=[b_f, b_rw], writes=[b_pR], inc=(kc == 7))
                    P.op("dve", lambda e, T=T: e.tensor_tensor(lg[:, T, :], pR[:, 0:20], rbb[:], ALU.add),
                         reads=[b_pR, b_rbb], writes=[b_lg])

                b2_pre(0)
                b2_pe1(0)
                pre_e = [0]
                for T in range(16):
                    if T + 1 < 16:
                        b2_pre(T + 1)
                    b2_back(T)
                    if T + 1 < 16:
                        b2_pe1(T + 1)
                    if T % 3 == 1 and pre_e[0] < NS:
                        load_expert(pre_e[0])
                        pre_e[0] += 1
                while pre_e[0] < NS:
                    load_expert(pre_e[0])
                    pre_e[0] += 1
                def sbt(name, shape, dt=F32):
                    return ph.sb(name, shape, dt)
                gmax, b_gmax = sbt("gmax", [128, 16])
                gsh, b_gsh = sbt("gsh", [128, 16, 4])
                gsum, b_gsum = sbt("gsum", [128, 16])
                gw, b_gw = sbt("gw", [128, 16])
                ohg, b_ohg = sbt("ohg", [128, 16, 4])
                tmp4, b_tmp4 = sbt("tmp4", [128, 16, 4, 4])
                esel, b_esel = sbt("esel", [128, 16, 4])
                m1, b_m1 = sbt("m1", [128, 16])
                mk1, b_mk1 = sbt("mk1", [128, 16, 4])
                e2, b_e2 = sbt("e2", [128, 16, 4])
                m2, b_m2 = sbt("m2", [128, 16])
                mk2, b_mk2 = sbt("mk2", [128, 16, 4])
                tt_, b_tt = sbt("tt", [128, 16])
                w1, b_w1 = sbt("w1", [128, 16])
                w2, b_w2 = sbt("w2", [128, 16])
                ing, b_ing = sbt("ing", [128, 16, 4])
                ing2, b_ing2 = sbt("ing2", [128, 16, 4])
                gates, b_gates = sbt("gates", [128, 16, 4, 4], BF16)
                gl = lg[:, :, 0:4]
                el = lg[:, :, 4:20].rearrange("p t (g e) -> p t g e", e=4)

                def bc3(ap2):
                    return ap2.unsqueeze(2).to_broadcast([128, 16, 4])
                V = lambda f, r, w: P.op("dve", f, reads=r, writes=w)
                V(lambda e: e.tensor_reduce(gmax[:], gl, AX.X, ALU.max), [b_lg], [b_gmax])
                V(lambda e: e.tensor_tensor(gsh[:], gl, bc3(gmax[:]), ALU.subtract), [b_lg, b_gmax], [b_gsh])
                V(lambda e: e.tensor_tensor(ohg[:], gl, bc3(gmax[:]), ALU.is_equal), [b_lg, b_gmax], [b_ohg])
                P.op("act", lambda e: e.activation(gsh[:], gsh[:], AF.Exp), reads=[b_gsh], writes=[b_gsh])
                V(lambda e: e.tensor_reduce(gsum[:], gsh[:], AX.X, ALU.add), [b_gsh], [b_gsum])
                V(lambda e: e.reciprocal(gw[:], gsum[:]), [b_gsum], [b_gw])
                V(lambda e: e.tensor_tensor(tmp4[:], el, ohg[:].unsqueeze(3).to_broadcast([128, 16, 4, 4]), ALU.mult),
                  [b_lg, b_ohg], [b_tmp4])
                V(lambda e: e.tensor_reduce(esel[:], tmp4[:].rearrange("p t g e -> p t e g"), AX.X, ALU.add), [b_tmp4], [b_esel])
                V(lambda e: e.tensor_reduce(m1[:], esel[:], AX.X, ALU.max), [b_esel], [b_m1])
                V(lambda e: e.tensor_tensor(mk1[:], esel[:], bc3(m1[:]), ALU.is_equal), [b_esel, b_m1], [b_mk1])
                V(lambda e: e.scalar_tensor_tensor(e2[:], mk1[:], -1e30, esel[:], ALU.mult, ALU.add), [b_mk1, b_esel], [b_e2])
                V(lambda e: e.tensor_reduce(m2[:], e2[:], AX.X, ALU.max), [b_e2], [b_m2])
                V(lambda e: e.tensor_tensor(mk2[:], e2[:], bc3(m2[:]), ALU.is_equal), [b_e2, b_m2], [b_mk2])
                V(lambda e: e.tensor_tensor(tt_[:], m2[:], m1[:], ALU.subtract), [b_m1, b_m2], [b_tt])
                P.op("act", lambda e: e.activation(tt_[:], tt_[:], AF.Exp), reads=[b_tt], writes=[b_tt])
                V(lambda e: e.tensor_scalar(w1[:], tt_[:], 1.0, None, ALU.add), [b_tt], [b_w1])
                V(lambda e: e.reciprocal(w1[:], w1[:]), [b_w1], [b_w1])
                V(lambda e: e.tensor_tensor(w1[:], w1[:], gw[:], ALU.mult), [b_w1, b_gw], [b_w1])
                V(lambda e: e.tensor_tensor(w2[:], w1[:], tt_[:], ALU.mult), [b_w1, b_tt], [b_w2])
                V(lambda e: e.tensor_tensor(ing[:], mk1[:], bc3(w1[:]), ALU.mult), [b_mk1, b_w1], [b_ing])
                V(lambda e: e.tensor_tensor(ing2[:], mk2[:], bc3(w2[:]), ALU.mult), [b_mk2, b_w2], [b_ing2])
                V(lambda e: e.tensor_tensor(ing[:], ing[:], ing2[:], ALU.add), [b_ing, b_ing2], [b_ing])
                V(lambda e: e.tensor_tensor(gates[:], ohg[:].unsqueeze(3).to_broadcast([128, 16, 4, 4]),
                                            ing[:].unsqueeze(2).to_broadcast([128, 16, 4, 4]), ALU.mult),
                  [b_ohg, b_ing], [b_gates])
                for T in range(16):
                    P.op("pe", lambda e, T=T: e.transpose(pGT[0:16, (T % 8) * 128:(T % 8 + 1) * 128],
                                                          gates[:, T].rearrange("p g e -> p (g e)"), identb[:]),
                         reads=[b_gates, b_identb], writes=[b_pGT], inc=(T % 8 == 7))
                    if T % 8 == 7:
                        P.op("dve", lambda e, T=T: e.tensor_copy(gT[:, (T - 7) * 128:(T + 1) * 128], pGT[0:16, :]),
                             reads=[b_pGT], writes=[b_gT])
                if stopB == "B2":
                    P.dma("sp", dbg["hfT"], hfT[:], reads=[b_hfT], writes=[b_out])
                    P.dma("sp", dbg["gT"], gT[:], reads=[b_gT], writes=[b_out])
                    P.dma("sp", dbg["lg"], lg[:], reads=[b_lg], writes=[b_out])
            if stopB == "B2":
                return
            with Phase(P, f"B3{l}") as ph:
                selm, b_selm = ph.sb("selm", [16, 16, 128], BF16)
                hid = [ph.sb("hid", [128, 8, 512], BF16) for _ in range(2)]
                Gsb = [ph.sb("Gsb", [128, 512], F32) for _ in range(1)]
                sg = [ph.sb("sg", [128, 512], F32) for _ in range(1)]
                t32 = [ph.sb("t32", [128, 512], F32) for _ in range(1)]
                pGb, b_pGb = ph.ps("pGb", [128, 512])
                pGa = [ph.ps("pGa", [128, 512]) for _ in range(2)]
                pUp = [ph.ps("pUp", [128, 512]) for _ in range(2)]
                pY2 = [ph.ps("pY2", [128, 1024]) for _ in range(1)]
                P.op("pool", lambda e: e.memset(selm[:], 1.0), writes=[b_selm])
                P.op("pool", lambda e: e.affine_select(selm[:], selm[:], [[-1, 16], [0, 128]], ALU.is_equal, 0.0,
                                                       base=0, channel_multiplier=1), reads=[b_selm], writes=[b_selm])
                cnt2 = 0
                for grp in range(4):
                    for tg in range(4):
                        h_t, b_h = hid[(grp * 4 + tg) % 2]
                        for ei in range(4):
                            e_ = grp * 4 + ei
                            s_ = e_ % NS
                            G_t, b_G = Gsb[0]
                            P.op("pe", lambda e, e_=e_, tg=tg: e.matmul(pGb[:, :], selm[0:16, e_, :], gT[0:16, tg * 512:(tg + 1) * 512],
                                                                         start=True, stop=True),
                                 reads=[b_selm, b_gT], writes=[b_pGb])
                            P.op("act", lambda e, G_t=G_t: e.activation(G_t[:], pGb[:], AF.Copy), reads=[b_pGb], writes=[b_G])
                            for fc in range(2):
                                ga_t, b_ga = pGa[cnt2 % 2]
                                up_t, b_up = pUp[cnt2 % 2]
                                s_t, b_s = sg[0]
                                t_t, b_t = t32[0]
                                cnt2 += 1
                                for kc in range(8):
                                    P.op("pe", lambda e, s_=s_, kc=kc, fc=fc, tg=tg, ga_t=ga_t: e.matmul(
                                        ga_t[:], wg[s_][0][:, kc, fc * 128:(fc + 1) * 128], hfT[:, kc, tg * 512:(tg + 1) * 512],
                                        start=(kc == 0), stop=(kc == 7)),
                                        reads=[wg[s_][1], b_hfT], writes=[b_ga], inc=(kc == 7))
                                for kc in range(8):
                                    P.op("pe", lambda e, s_=s_, kc=kc, fc=fc, tg=tg, up_t=up_t: e.matmul(
                                        up_t[:], wu[s_][0][:, kc, fc * 128:(fc + 1) * 128], hfT[:, kc, tg * 512:(tg + 1) * 512],
                                        start=(kc == 0), stop=(kc == 7)),
                                        reads=[wu[s_][1], b_hfT], writes=[b_up], inc=(kc == 7))
                                P.op("act", lambda e, s_t=s_t, ga_t=ga_t: e.activation(s_t[:], ga_t[:], AF.Silu), reads=[b_ga], writes=[b_s])
                                P.op("dve", lambda e, s_t=s_t, up_t=up_t, t_t=t_t: e.tensor_tensor(t_t[:], s_t[:], up_t[:], ALU.mult),
                                     reads=[b_s, b_up], writes=[b_t])
                                P.op("pool", lambda e, t_t=t_t, G_t=G_t, h_t=h_t, c=ei * 2 + fc: e.tensor_tensor(h_t[:, c, :], t_t[:], G_t[:], ALU.mult),
                                     reads=[b_t, b_G], writes=[b_h])
                        for t4 in range(4):
                            T = tg * 4 + t4
                            y_t, b_y = pY2[0]
                            for half in range(2):
                                for c in range(8):
                                    s_ = (grp * 4 + c // 2) % NS
                                    P.op("pe", lambda e, c=c, s_=s_, t4=t4, half=half, h_t=h_t, y_t=y_t: e.matmul(
                                        y_t[:, half * 512:(half + 1) * 512], h_t[:, c, t4 * 128:(t4 + 1) * 128],
                                        wd[s_][0][:, c % 2, half * 512:(half + 1) * 512], start=(c == 0), stop=(c == 7)),
                                        reads=[b_h, wd[s_][1]], writes=[b_y], inc=(c == 7 and half == 1))
                            P.op("dve", lambda e, T=T, y_t=y_t: e.tensor_tensor(xres[:, T, :], xres[:, T, :], y_t[:], ALU.add),
                                 reads=[b_y, b_xr[T]], writes=[b_xr[T]])
                    for e_ in range(NS + grp * 4, min(16, NS + grp * 4 + 4)):
                        load_expert(e_)
            if stopB == "B3":
                P.dma("sp", dbg["x"], xres[:], reads=b_xr, writes=[b_out])
                return
            if l == 1:
                for T in range(16):
                    P.dma("sp", out[T * 128:(T + 1) * 128, :], xres[:, T, :], reads=[b_xr[T]], writes=[b_out])
                return
            with Phase(P, f"B4{l}") as ph:
                Abc, b_Abc = ph.sb("Abc", [128, D], F32)
                Sbc, b_Sbc = ph.sb("Sbc", [128, D], F32)
                tmod, b_tmod = ph.sb("tmod", [128, D], F32)
                junk, b_junk = ph.sb("junk", [128, D], BF16)
                hmb = [ph.sb("hmb", [128, D], BF16) for _ in range(2)]
                hT = [ph.sb("hT", [128, 8, 128], BF16) for _ in range(2)]
                identb, b_identb = ph.sb("identb", [128, 128], BF16)
                ssx, b_ssx = ph.sb("ssx", [128, 16], F32)
                rsx, b_rsx = ph.sb("rsx", [128, 16], F32)
                pX = [ph.ps("pX", [128, 8, 128], BF16) for _ in range(2)]
                P.op("pool", lambda e: e.memset(identb[:], 1.0), writes=[b_identb])
                P.op("pool", lambda e: e.affine_select(identb[:], identb[:], [[-1, 128]], ALU.is_equal, 0.0,
                                                       base=0, channel_multiplier=1), reads=[b_identb], writes=[b_identb])
                P.op("dve", lambda e: e.memset(ssx[:], 0.0), writes=[b_ssx])
                P.dma("sp", Sbc[:], mr[1:2, 0:D].to_broadcast([128, D]), reads=[b_modrow], writes=[b_Sbc])
                P.dma("sp", Abc[:], mr[1:2, D:2 * D].to_broadcast([128, D]), reads=[b_modrow], writes=[b_Abc])
                P.dma("sp", tmod[:], norm_mix[1:2, :].to_broadcast([128, D]), writes=[b_tmod])
                P.op("dve", lambda e: e.scalar_tensor_tensor(Abc[:], Abc[:], 1.0, tmod[:], ALU.add, ALU.mult),
                     reads=[b_Abc, b_tmod], writes=[b_Abc])
                def b4_pre(T):
                    P.dma("pool", xmid.ap()[T * 128:(T + 1) * 128, :], xres[:, T, :], reads=[b_xr[T]], writes=[b_xmid])
                    m_t, b_m = hmb[T % 2]
                    P.op("act", lambda e, T=T: e.activation(junk[:], xres[:, T, :], AF.Square, accum_out=ssx[:, T:T + 1]),
                         reads=[b_xr[T]], writes=[b_junk, b_ssx])
                    P.op("act", lambda e, T=T: e.activation(rsx[:, T:T + 1], ssx[:, T:T + 1], AF.Ln, bias=EPS, scale=1.0 / D),
                         reads=[b_ssx], writes=[b_rsx])
                    P.op("act", lambda e, T=T: e.activation(rsx[:, T:T + 1], rsx[:, T:T + 1], AF.Exp, scale=-0.5), reads=[b_rsx], writes=[b_rsx])
                    P.op("dve", lambda e, T=T: e.scalar_tensor_tensor(tmod[:], xres[:, T, :], rsx[:, T:T + 1], Abc[:], ALU.mult, ALU.mult),
                         reads=[b_xr[T], b_rsx, b_Abc], writes=[b_tmod])
                    P.op("pool", lambda e, m_t=m_t: e.tensor_tensor(m_t[:], tmod[:], Sbc[:], ALU.add),
                         reads=[b_tmod, b_Sbc], writes=[b_m])

                def b4_pe(T):
                    m_t, b_m = hmb[T % 2]
                    x_t, b_xT = pX[T % 2]
                    for kc in range(8):
                        P.op("pe", lambda e, kc=kc, m_t=m_t, x_t=x_t: e.transpose(x_t[:, kc, :], m_t[:, kc * 128:(kc + 1) * 128], identb[:]),
                             reads=[b_m, b_identb], writes=[b_xT], inc=(kc == 7))

                def b4_back(T):
                    x_t, b_xT = pX[T % 2]
                    o_t, b_o = hT[T % 2]
                    P.op("act", lambda e, x_t=x_t, o_t=o_t: e.activation(o_t[:], x_t[:], AF.Copy), reads=[b_xT], writes=[b_o])
                    for c in range(4):
                        P.dma("sp", sendH[c].ap()[:, T * 128:(T + 1) * 128].rearrange("(k p) t -> p k t", p=128), o_t[:, 2 * c:2 * c + 2, :],
                              reads=[b_o], writes=[b_sendH[c]])

                b4_pre(0)
                b4_pe(0)
                for T in range(16):
                    if T + 1 < 16:
                        b4_pre(T + 1)
                    b4_back(T)
                    if T + 1 < 16:
                        b4_pe(T + 1)

    if stop == "X1":
        dbg["rA"] = dout("dbg_rA", [1024, S], BF16)
        for c in range(4):
            P.collective("AllGather", sendA[c], recvA[c], reads=[b_sendA[c]], writes=[b_recvA[c]])
            for rk in range(2):
                P.dma("sp", dbg["rA"][rk * 512 + c * 128: rk * 512 + (c + 1) * 128, :], recvA[c].ap()[rk * 128:(rk + 1) * 128, :],
                      reads=[b_recvA[c]], writes=[b_out])
        P.barrier()
        P.flush()
        P.stack.close()
        return nc
    if stop in ("B1", "B2", "B3", "B"):
        if stop in ("B1", "B3", "B"):
            dbg["x"] = dout("dbg_x", [128, 16, D])
        if stop == "B2":
            dbg["hfT"] = dout("dbg_hfT", [128, 8, OWN], BF16)
            dbg["gT"] = dout("dbg_gT", [16, OWN], BF16)
            dbg["lg"] = dout("dbg_lg", [128, 16, 20])
        if stop == "B":
            dbg["hT"] = dout("dbg_hT", [1024, OWN], BF16)
    phase_B(0, sendA, b_sendA, recvA, b_recvA, a_w_out, stopB=(stop if stop in ("B1", "B2", "B3") else None))
    if stop in ("B1", "B2", "B3", "B"):
        if stop == "B":
            P.dma("sp", dbg["x"], xmid.ap().rearrange("(t p) d -> p t d", p=128), reads=[b_xmid], writes=[b_out])
            for c in range(4):
                P.dma("sp", dbg["hT"][c * 256:(c + 1) * 256, :], sendH[c].ap(), reads=[b_sendH[c]], writes=[b_out])
        P.barrier()
        P.flush()
        P.stack.close()
        return nc

    with Phase(P, "C") as ph:
        KT1, _ = ph.sb("KT1", [128, 4, S], BF16)
        b_KT = [P.buf(f"KT{q}") for q in range(8)]
        Vp1, _ = ph.sb("Vp1", [128, 32, 8, 128], BF16)
        b_Vp = [P.buf(f"Vp{q}") for q in range(8)]
        cumK, _ = ph.sb("cumK", [128, 32, 8], F32)
        b_cum = [P.buf(f"cum{q}") for q in range(8)]
        QT1 = [ph.sb("QT1", [128, 4, 512], BF16) for _ in range(2)]
        QT1b = [ph.sb("QT1b", [128, 4, 512], BF16) for _ in range(2)]
        hmc = [ph.sb("hmc", [128, 8, 512], BF16) for _ in range(2)]
        wC, b_wC = ph.sb("wC", [128, 8, 1544], BF16)
        wstg = [ph.sb("wstg", [128, 1544], F32) for _ in range(2)]
        ident, b_ident = ph.sb("ident", [128, 128], BF16)
        tri, b_tri = ph.sb("tri", [128, 128], BF16)
        U, b_U = ph.sb("U", [128, 128], F32)
        onesf, b_onesf = ph.sb("onesf", [128, 128], F32)
        gq, b_gq_ = ph.sb("gq", [128, 1], F32)
        gk, b_gk_ = ph.sb("gk", [128, 1], F32)
        fbb, b_fbb = ph.sb("fbb", [128, 8], F32)
        sq, b_sq = ph.sb("sq", [128, 1024], F32)
        ssq, b_ssq = ph.sb("ssq", [128, 16], F32)
        rsq, b_rsq = ph.sb("rsq", [128, 16], F32)
        qkn2 = [ph.sb("qkn", [128, 1024], BF16) for _ in range(2)]
        zf2 = [ph.sb("zf", [128, 8], F32) for _ in range(2)]
        lfn2 = [ph.sb("lfn", [128, 8], F32) for _ in range(2)]
        carry, b_carry = ph.sb("carry", [1, 8], F32)
        cref, b_cref = ph.sb("cref", [1, 8], F32)
        biasq = [ph.sb("biasq", [128, 32, 8], F32) for _ in range(2)]
        PT = [ph.sb("PT", [128, 512], BF16) for _ in range(3)]
        rec, b_rec = ph.sb("rec", [128, 512], F32)
        oTc = [ph.sb("oTc", [64, 512], BF16) for _ in range(2)]
        pA, b_pA = ph.ps("pA", [128, 1024])
        pT, b_pT = ph.ps("pT", [128, 8, 128], BF16)
        pS = [ph.ps("pS", [128, 512]) for _ in range(3)]
        pO, b_pO = ph.ps("pO", [128, 512])
        pB, b_pB = pA[:, 0:512], b_pA
        pMi, b_pMi = ph.ps("pMi", [128, 512])

        P.op("pool", lambda e: e.memset(ident[:], 1.0), writes=[b_ident])
        P.op("pool", lambda e: e.affine_select(ident[:], ident[:], [[-1, 128]], ALU.is_equal, 0.0,
                                               base=0, channel_multiplier=1), reads=[b_ident], writes=[b_ident])
        P.op("pool", lambda e: e.memset(tri[:], 1.0), writes=[b_tri])
        P.op("pool", lambda e: e.affine_select(tri[:], tri[:], [[1, 128]], ALU.is_ge, 0.0,
                                               base=0, channel_multiplier=-1), reads=[b_tri], writes=[b_tri])
        P.op("pool", lambda e: e.memset(U[:], 1.0), writes=[b_U])
        P.op("pool", lambda e: e.affine_select(U[:], U[:], [[1, 128]], ALU.is_ge, 0.0,
                                               base=0, channel_multiplier=-1), reads=[b_U], writes=[b_U])
        P.op("pool", lambda e: e.memset(onesf[:], 1.0), writes=[b_onesf])
        P.op("dve", lambda e: e.memset(Vp1[:], 1.0), writes=b_Vp)
        P.op("dve", lambda e: e.memset(carry[:], 0.0), writes=[b_carry])
        for i in range(2):
            P.op("dve", lambda e, i=i: e.memset(QT1[i][0][:], 0.0), writes=[QT1[i][1]])
            P.op("dve", lambda e, i=i: e.memset(QT1b[i][0][:], 0.0), writes=[QT1[i][1]])
        P.dma("sp", gq[:], b_gq, writes=[b_gq_])
        P.dma("sp", gk[:], b_gk, writes=[b_gk_])
        P.dma("sp", fbb[:], b_fb[0:1, :].to_broadcast([128, 8]), writes=[b_fbb])
        for kc in range(8):
            s_t, b_s = wstg[kc % 2]
            P.dma("sp", s_t[:], b_w_in[kc * 128:(kc + 1) * 128, :], writes=[b_s])
            P.op("dve", lambda e, s_t=s_t, kc=kc: e.tensor_copy(wC[:, kc, :], s_t[:]), reads=[b_s], writes=[b_wC])
        for c in range(4):
            P.collective("AllGather", sendH[c], recvH[c], reads=[b_sendH[c]], writes=[b_recvH[c]])

        def project(qg):
            h_t, b_h = hmc[qg % 2]
            q_t, b_q = QT1[qg % 2]
            rank, col0 = qg // 4, (qg % 4) * 512
            for kc in range(8):
                c = kc // 2
                r0 = rank * 256 + (kc % 2) * 128
                P.dma("sp", h_t[:, kc, :], recvH[c].ap()[r0:r0 + 128, col0:col0 + 512], reads=[b_recvH[c]], writes=[b_h])

            def st_qk(tb4):
                tsl = slice(tb4 * 128, (tb4 + 1) * 128)
                qn_t, b_qn = qkn2[tb4 % 2]
                for kc in range(8):
                    for half in range(2):
                        P.op("pe", lambda e, kc=kc, half=half, tsl=tsl: e.matmul(
                            pA[:, half * 512:(half + 1) * 512], h_t[:, kc, tsl], wC[:, kc, half * 512:(half + 1) * 512],
                            start=(kc == 0), stop=(kc == 7)),
                            reads=[b_h, b_wC], writes=[b_pA], inc=(kc == 7 and half == 1))
                P.op("act", lambda e: e.activation(sq[:], pA[:], AF.Square), reads=[b_pA], writes=[b_sq])
                P.op("dve", lambda e: e.tensor_reduce(ssq[:], sq[:].rearrange("p (h d) -> p h d", d=64), AX.X, ALU.add),
                     reads=[b_sq], writes=[b_ssq])
                P.op("act", lambda e: e.activation(rsq[:], ssq[:], AF.Ln, bias=EPS, scale=1.0 / 64), reads=[b_ssq], writes=[b_rsq])
                P.op("act", lambda e: e.activation(rsq[:], rsq[:], AF.Exp, scale=-0.5), reads=[b_rsq], writes=[b_rsq])
                P.op("dve", lambda e, qn_t=qn_t: e.tensor_tensor(qn_t[:].rearrange("p (h d) -> p h d", d=64),
                                                      pA[:].rearrange("p (h d) -> p h d", d=64),
                                                      rsq[:].unsqueeze(2).to_broadcast([128, 16, 64]), ALU.mult),
                     reads=[b_pA, b_rsq], writes=[b_qn])

            def st_vf(tb4):
                tb = 4 * qg + tb4
                tsl = slice(tb4 * 128, (tb4 + 1) * 128)
                z_t, b_z = zf2[tb4 % 2]
                l_t, b_l = lfn2[tb4 % 2]
                pv_t, b_pv = pS[0]
                pf_t, b_pf = pS[1]
                for kc in range(8):
                    P.op("pe", lambda e, kc=kc, tsl=tsl: e.matmul(pv_t[:, 0:512], h_t[:, kc, tsl], wC[:, kc, 1024:1536],
                                                                 start=(kc == 0), stop=(kc == 7)),
                         reads=[b_h, b_wC], writes=[b_pv], inc=False)
                    P.op("pe", lambda e, kc=kc, tsl=tsl: e.matmul(pf_t[:, 0:8], h_t[:, kc, tsl], wC[:, kc, 1536:1544],
                                                                 start=(kc == 0), stop=(kc == 7)),
                         reads=[b_h, b_wC], writes=[b_pf], inc=(kc == 7))
                P.op("act", lambda e, tb=tb: e.activation(Vp1[:, tb, :, 0:64], pv_t[:, 0:512].rearrange("p (h d) -> p h d", d=64), AF.Copy),
                     reads=[b_pv], writes=[b_Vp[qg]])
                P.op("dve", lambda e, z_t=z_t: e.tensor_tensor(z_t[:], pf_t[:, 0:8], fbb[:], ALU.add), reads=[b_pf, b_fbb], writes=[b_z])
                P.op("act", lambda e, z_t=z_t: e.activation(z_t[:], z_t[:], AF.Exp, scale=-1.0), reads=[b_z], writes=[b_z])
                P.op("act", lambda e, z_t=z_t: e.activation(z_t[:], z_t[:], AF.Ln, bias=1.0), reads=[b_z], writes=[b_z])
                P.op("dve", lambda e, z_t=z_t, l_t=l_t: e.tensor_scalar(l_t[:], z_t[:], -1.0, None, ALU.mult), reads=[b_z], writes=[b_l])

            def st_cs(tb4):
                tb = 4 * qg + tb4
                l_t, b_l = lfn2[tb4 % 2]
                P.op("pe", lambda e, l_t=l_t: e.matmul(pMi[:, 0:8], U[:], l_t[:], start=True, stop=False),
                     reads=[b_U, b_l], writes=[b_pMi], inc=False)
                P.op("pe", lambda e: e.matmul(pMi[:, 0:8], onesf[0:1, :], carry[0:1, :], start=False, stop=True),
                     reads=[b_onesf, b_carry], writes=[b_pMi], inc=False)
                P.op("pe", lambda e, l_t=l_t: e.matmul(pMi[0:1, 8:16], onesf[:, 0:1], l_t[:], start=True, stop=False),
                     reads=[b_onesf, b_l], writes=[b_pMi], inc=False)
                P.op("pe", lambda e: e.matmul(pMi[0:1, 8:16], onesf[0:1, 0:1], carry[0:1, :], start=False, stop=True),
                     reads=[b_onesf, b_carry], writes=[b_pMi], inc=True)
                P.op("dve", lambda e, tb=tb: e.tensor_copy(cumK[:, tb, :], pMi[:, 0:8]), reads=[b_pMi], writes=[b_cum[qg]])
                P.op("dve", lambda e: e.tensor_copy(carry[:], pMi[0:1, 8:16]), reads=[b_pMi], writes=[b_carry])
                if tb4 == 1:
                    P.op("dve", lambda e: e.tensor_copy(cref[:], pMi[0:1, 8:16]), reads=[b_pMi], writes=[b_cref])

            def st_tr(tb4):
                tb = 4 * qg + tb4
                tsl = slice(tb4 * 128, (tb4 + 1) * 128)
                qn_t, b_qn = qkn2[tb4 % 2]
                for c8 in range(8):
                    P.op("pe", lambda e, c8=c8, qn_t=qn_t: e.transpose(pT[:, c8, :], qn_t[:, c8 * 128:(c8 + 1) * 128], ident[:]),
                         reads=[b_qn, b_ident], writes=[b_pT], inc=(c8 == 7))
                qb_t = QT1b[qg % 2][0]
                P.op("act", lambda e, tsl=tsl: e.activation(q_t[0:64, :, tsl], pT[0:64, 0:4, :], AF.Copy, scale=gq[0:64, 0:1]),
                     reads=[b_pT, b_gq_], writes=[b_q])
                P.op("act", lambda e, qb_t=qb_t, tsl=tsl: e.activation(qb_t[64:128, :, tsl], pT[64:128, 0:4, :], AF.Copy, scale=gq[64:128, 0:1]),
                     reads=[b_pT, b_gq_], writes=[b_q])
                P.op("dve", lambda e, tb=tb: e.tensor_scalar(KT1[:, :, tb * 128:(tb + 1) * 128], pT[:, 4:8, :], gk[:, 0:1], None, ALU.mult),
                     reads=[b_pT, b_gk_], writes=[b_KT[qg]])

            st_qk(0)
            st_vf(0)
            for tb4 in range(4):
                if tb4 + 1 < 4:
                    st_qk(tb4 + 1)
                    st_vf(tb4 + 1)
                st_cs(tb4)
                st_tr(tb4)
            bq_t, b_bq = biasq[qg % 2]
            nkb = 4 * qg + 4
            P.op("pe", lambda e: e.matmul(pMi[:, 16:24], onesf[0:1, :], cref[0:1, :], start=True, stop=True),
                 reads=[b_onesf, b_cref], writes=[b_pMi])
            P.op("dve", lambda e, bq_t=bq_t, nkb=nkb: e.tensor_tensor(bq_t[:, 0:nkb, :], pMi[:, 16:24].unsqueeze(1).to_broadcast([128, nkb, 8]),
                                                                      cumK[:, 0:nkb, :], ALU.subtract),
                 reads=[b_pMi] + b_cum[0:qg + 1], writes=[b_bq])

        cntS = [0]
        ko = [0]

        def attend(qg):
            q_t, b_q = QT1[qg % 2]
            bq_t, b_bq = biasq[qg % 2]
            nkb = 4 * qg + 4
            for h in range(8):
                pr, hp = h // 2, h % 2
                lo, hi = 64 * hp, 64 * hp + 64
                items = []
                for kb in range(nkb):
                    i = kb - 4 * qg
                    c0 = 128 * max(i, 0)
                    items.append((kb, i, c0))

                def s_mm(idx):
                    kb, i, c0 = items[idx]
                    s_t, b_s = pS[(cntS[0] + idx) % 3]
                    qz = q_t if hp == 0 else QT1b[qg % 2][0]
                    P.op("pe", lambda e, kb=kb, c0=c0, s_t=s_t, pr=pr, qz=qz: e.matmul(s_t[:, c0:512], KT1[:, pr, kb * 128:(kb + 1) * 128],
                                                                       qz[:, pr, c0:512], start=True, stop=True),
                         reads=[b_KT[kb // 4], b_q], writes=[b_s])

                s_mm(0)
                if len(items) > 1:
                    s_mm(1)
                for idx, (kb, i, c0) in enumerate(items):
                    if idx + 2 < len(items):
                        s_mm(idx + 2)
                    s_t, b_s = pS[(cntS[0] + idx) % 3]
                    p_t, b_p = PT[(cntS[0] + idx) % 3]
                    P.op("act", lambda e, kb=kb, c0=c0, s_t=s_t, p_t=p_t, h=h: e.activation(p_t[:, c0:512], s_t[:, c0:512], AF.Exp,
                                                                                      bias=bq_t[:, kb, h:h + 1], scale=0.125),
                         reads=[b_s, b_bq], writes=[b_p])
                    if i >= 0:
                        P.op("pool", lambda e, c0=c0, p_t=p_t: e.tensor_tensor(p_t[:, c0:c0 + 128], p_t[:, c0:c0 + 128], tri[:], ALU.mult),
                             reads=[b_p, b_tri], writes=[b_p])
                    P.op("pe", lambda e, kb=kb, c0=c0, p_t=p_t, idx=idx, h=h, n_=len(items): e.matmul(pO[:, c0:512], Vp1[:, kb, h, :], p_t[:, c0:512],
                                                                                start=(idx == 0), stop=(idx == n_ - 1)),
                         reads=[b_Vp[kb // 4], b_p], writes=[b_pO], inc=(idx == len(items) - 1))
                cntS[0] += len(items)
                o_t, b_o = oTc[ko[0] % 2]
                ko[0] += 1
                P.op("dve", lambda e: e.reciprocal(rec[64:65, :], pO[64:65, :]), reads=[b_pO], writes=[b_rec])
                P.op("pe", lambda e: e.matmul(pB[0:64, :], onesf[64:65, 0:64], rec[64:65, :], start=True, stop=True),
                     reads=[b_onesf, b_rec], writes=[b_pB])
                P.op("act", lambda e: e.activation(rec[0:64, :], pB[0:64, :], AF.Copy), reads=[b_pB], writes=[b_rec])
                P.op("dve", lambda e, o_t=o_t: e.tensor_tensor(o_t[:], pO[0:64, :], rec[0:64, :], ALU.mult),
                     reads=[b_pO, b_rec], writes=[b_o])
                row = (h % 2) * 64
                P.dma("sp", sendC[h // 2].ap()[row:row + 64, qg * 512:(qg + 1) * 512], o_t[:], reads=[b_o], writes=[b_sendC[h // 2]])

        project(0)
        for qg in range(8):
            if qg + 1 < 8:
                project(qg + 1)
            attend(qg)

    if stop == "C":
        dbg["oC"] = dout("dbg_oC", [512, S], BF16)
        for c in range(4):
            P.dma("sp", dbg["oC"][c * 128:(c + 1) * 128, :], sendC[c].ap(), reads=[b_sendC[c]], writes=[b_out])
        P.barrier()
        P.flush()
        P.stack.close()
        return nc

    phase_B(1, sendC, b_sendC, recvC, b_recvC, b_w_out)
    P.barrier()
    P.flush()
    P.stack.close()
    return nc


def make_in_maps(inp, small=False):
    f = lambda a: np.ascontiguousarray(np.asarray(a, dtype=np.float32))
    x = f(inp["x"]); c = f(inp["c"])
    a_w_in = f(inp["a_w_in"])[0].reshape(D, 3, 3, 16, 64)
    b_w_in = f(inp["b_w_in"])[0]
    rel = f(inp["rel_bias"]).reshape(32, 3, 16)
    oh = t5_onehot()
    r_w = f(np.concatenate([inp["router_group_w"], inp["router_expert_w"]], axis=2))
    r_b = f(np.concatenate([inp["router_group_b"], inp["router_expert_b"]], axis=1))
    maps = []
    for core in range(8):
        b, hh = core // 2, core % 2
        hs = slice(hh * 8, hh * 8 + 8)
        awi = np.empty((D, 2, 3, 768), np.float32)
        for p in range(2):
            for g in range(3):
                for s_ in range(3):
                    awi[:, p, g, s_ * 256:(s_ + 1) * 256] = a_w_in[:, g, s_, hh * 8 + 4 * p: hh * 8 + 4 * p + 4, :].reshape(D, 256)
        bwi = np.concatenate([b_w_in[:, s_ * 1024 + hh * 512: s_ * 1024 + hh * 512 + 512] for s_ in range(3)]
                             + [b_w_in[:, 3072 + hh * 8: 3072 + hh * 8 + 8]], axis=1)
        selv = np.zeros((128, 2), np.float32); selv[:, hh] = 1.0
        m = dict(
            x_b=x[b], x_own=f(x[b, hh * OWN:(hh + 1) * OWN]),
            c_col=f(c[b].reshape(8, 128).T),
            w_ada=f(inp["w_ada"]), b_ada=f(inp["b_ada"]),
            norm_mix=f(inp["norm_mix"]), norm_ffn=f(inp["norm_ffn"]),
            relb=f(rel[:, :, hs]), ohc=oh,
            a_w_in=awi, a_w_out=f(inp["a_w_out"])[0],
            a_gq=f(np.tile(f(inp["a_q_norm"])[0], 2)[:, None]), a_gk=f(np.tile(f(inp["a_k_norm"])[0], 2)[:, None]),
            b_w_in=f(bwi), b_fb=f(f(inp["b_f_bias"])[:, hs]), b_w_out=f(inp["b_w_out"])[0],
            b_gq=f(np.tile(f(inp["b_q_norm"])[0], 2)[:, None]), b_gk=f(np.tile(f(inp["b_k_norm"])[0], 2)[:, None]),
            r_w=r_w, r_b=r_b,
            w_gate=f(inp["w_gate"]) if not small else np.zeros((1, 1, 8, 8), np.float32),
            w_up=f(inp["w_up"]) if not small else np.zeros((1, 1, 8, 8), np.float32),
            w_down=f(inp["w_down"]) if not small else np.zeros((1, 1, 8, 8), np.float32),
            sel=selv,
        )
        maps.append(m)
    return maps


_NC_CACHE = {}


def kernel(**inputs):
    maps = make_in_maps(inputs)
    if "nc" not in _NC_CACHE:
        _NC_CACHE["nc"] = build()
    res = run_bass_kernel_spmd(_NC_CACHE["nc"], maps, core_ids=list(range(8)))
    outp = np.empty((4, S, D), np.float32)
    for core in range(8):
        b, hh = core // 2, core % 2
        outp[b, hh * OWN:(hh + 1) * OWN] = res.results[core]["out"]
    return outp
```

```python
import contextlib
import numpy as np
import concourse.bass as bass
import concourse.mybir as mybir
from concourse.bass_utils import run_bass_kernel_spmd

F32 = mybir.dt.float32
BF16 = mybir.dt.bfloat16
ALU = mybir.AluOpType
AF = mybir.ActivationFunctionType
AX = mybir.AxisListType

S = 4096
D = 1024
OWN = 2048
EPS = 1e-6
DIL = (1, 4, 16)
PAIRS = [[0, 1], [2, 3], [4, 5], [6, 7]]
COMPUTE = ("pe", "act", "dve", "pool")


class Buf:
    def __init__(self, name, excl=False):
        self.name = name
        self.writers = {}
        self.readers = {}
        self.excl = excl
        self.sem = {}


class Prog:
    def __init__(self, nc):
        self.nc = nc
        self.stack = contextlib.ExitStack()
        self.q = {e: [] for e in ("pe", "act", "dve", "pool", "sp")}
        self.sems = {}
        self.cnt = {}
        self.known = {e: {} for e in self.q}
        self.pending = {e: False for e in self.q}
        for e in COMPUTE:
            self.sems[e] = self.stack.enter_context(nc.semaphore("prog_" + e))
            self.cnt[e] = 0
        self.nsem = 0
        self.nbuf = 0
        self.dmasems = []
        self.free_sems = {}
        self.phase_bufs = []

    def buf(self, name=None, excl=False):
        self.nbuf += 1
        b = Buf(name or f"b{self.nbuf}", excl)
        if self.phase_bufs:
            self.phase_bufs[-1].append(b)
        return b

    def dma_sem(self, kind):
        fl = self.free_sems.setdefault(kind, [])
        if fl:
            return fl.pop()
        sem = self.new_sem("dma" + kind)
        self.cnt[sem] = 0
        self.dmasems.append(sem)
        return sem

    def begin_phase(self):
        self.phase_bufs.append([])

    def end_phase(self):
        for b in self.phase_bufs.pop():
            for kind, sem in b.sem.items():
                self.free_sems.setdefault(kind, []).append(sem)
            b.sem = {}

    def new_sem(self, name):
        self.nsem += 1
        return self.stack.enter_context(self.nc.semaphore(f"{name}_{self.nsem}"))

    def _deps(self, eng, reads, writes):
        deps = {}

        def need(key, val):
            if deps.get(key, 0) < val:
                deps[key] = val

        for b in reads:
            for k, v in b.writers.items():
                need(k, v)
            if b.excl:
                for k, v in b.readers.items():
                    need(k, v)
        for b in writes:
            for k, v in b.writers.items():
                need(k, v)
            for k, v in b.readers.items():
                need(k, v)
        out = []
        kn = self.known[eng]
        for k, v in deps.items():
            if k == eng and eng == "pe":
                continue
            if kn.get(k, 0) >= v:
                continue
            kn[k] = v
            out.append((k, v))
        return out

    def _semof(self, key):
        return self.sems[key] if isinstance(key, str) else key

    def op(self, eng, fn, reads=(), writes=(), inc=True):
        waits = [(self._semof(k), v) for k, v in self._deps(eng, reads, writes)]
        if inc:
            self.cnt[eng] += 1
            self.pending[eng] = False
            ticket = self.cnt[eng]
        else:
            ticket = self.cnt[eng] + 1
            self.pending[eng] = True
        sem = self.sems[eng]

        def emit(e, waits=waits, fn=fn, inc=inc, sem=sem):
            for s, v in waits:
                e.wait_ge(s, v)
            ins = fn(e)
            if inc:
                ins.then_inc(sem, 1)

        self.q[eng].append(emit)
        for b in reads:
            b.readers[eng] = ticket
        for b in writes:
            b.writers[eng] = ticket
        return ticket

    def dma(self, eng, out_ap, in_ap, reads=(), writes=(), **kw):
        assert len(writes) == 1
        wb = writes[0]
        kind = "sw" if eng == "pool" else "hw"
        if kind not in wb.sem:
            wb.sem[kind] = self.dma_sem(kind)
        waits = [(self._semof(k), v) for k, v in self._deps(eng, reads, writes)]
        sem = wb.sem[kind]
        self.cnt[sem] += 16
        ticket = self.cnt[sem]

        def emit(e, waits=waits, sem=sem, out_ap=out_ap, in_ap=in_ap, kw=kw):
            for s, v in waits:
                e.wait_ge(s, v)
            e.dma_start(out=out_ap, in_=in_ap, **kw).then_inc(sem, 16)

        self.q[eng].append(emit)
        for b in reads:
            b.readers[sem] = ticket
        wb.writers[sem] = ticket
        return ticket

    def collective(self, kind, in_t, out_t, reads, writes):
        eng = "pool"
        wb = writes[0]
        waits = [(self._semof(k), v) for k, v in self._deps(eng, reads, writes)]
        sem = self.new_sem("cc")
        self.cnt[sem] = 1

        def emit(e, waits=waits, sem=sem):
            for s, v in waits:
                e.wait_ge(s, v)
            e.collective_compute(kind, ALU.bypass, replica_groups=PAIRS,
                                 ins=[in_t.ap().opt()], outs=[out_t.ap().opt()]).then_inc(sem)
            e.wait_ge(sem, 1)

        self.q[eng].append(emit)
        for b in reads:
            b.readers[sem] = 1
        wb.writers[sem] = 1
        self.known[eng][sem] = 1

    def barrier(self):
        for eng in self.q:
            waits = []
            for k in list(COMPUTE) + self.dmasems:
                v = self.cnt[k]
                if v == 0 or k == eng:
                    continue
                if self.known[eng].get(k, 0) >= v:
                    continue
                self.known[eng][k] = v
                waits.append((self._semof(k), v))

            def emit(e, waits=waits):
                for s, v in waits:
                    e.wait_ge(s, v)

            self.q[eng].append(emit)

    def flush(self):
        nc = self.nc
        for eng in COMPUTE:
            if self.pending[eng]:
                raise RuntimeError(f"pending un-inc'd op on {eng}")
        q = self.q
        self.q = {e: [] for e in q}
        with nc.Block() as block:
            @block.tensor
            def _(e):
                for f in q["pe"]:
                    f(e)

            @block.scalar
            def _(e):
                for f in q["act"]:
                    f(e)

            @block.vector
            def _(e):
                for f in q["dve"]:
                    f(e)

            @block.gpsimd
            def _(e):
                for f in q["pool"]:
                    f(e)

            @block.sync
            def _(e):
                for f in q["sp"]:
                    f(e)


class Phase:
    def __init__(self, P, name):
        self.P = P
        self.nc = P.nc
        self.name = name
        self.stack = contextlib.ExitStack()
        self.n = 0

    def __enter__(self):
        self.stack.__enter__()
        self.P.begin_phase()
        return self

    def __exit__(self, *a):
        self.P.barrier()
        self.P.flush()
        self.P.end_phase()
        return self.stack.__exit__(*a)

    def sb(self, name, shape, dtype):
        self.n += 1
        t = self.stack.enter_context(self.nc.sbuf_tensor(f"{self.name}_{name}_{self.n}", list(shape), dtype))
        return t, self.P.buf(name)

    def ps(self, name, shape, dtype=F32):
        self.n += 1
        t = self.stack.enter_context(self.nc.psum_tensor(f"{self.name}_{name}_{self.n}", list(shape), dtype))
        return t, self.P.buf(name, excl=True)


def t5_onehot():
    oh = np.zeros((32, 3, 384), np.float32)
    for g, r in enumerate(DIL):
        for dist in range(0, 129):
            d = np.int32(dist * r)
            if d < 16:
                b = int(d)
            else:
                v = np.log(np.float32(max(d, 1)) / np.float32(16)) / np.float32(np.log(2048 / 16)) * np.float32(16)
                b = min(16 + int(np.float32(v)), 31)
            oh[b, g, 127 + dist] = 1.0
    return oh


def build(stop=None, nsteps=None, do2=True, donorm=True):
    nc = bass.Bass("TRN2", target_bir_lowering=False)

    def din(name, shape, dt=F32):
        return nc.dram_tensor(name, list(shape), dt, kind="ExternalInput").ap()

    def dout(name, shape, dt=F32):
        return nc.dram_tensor(name, list(shape), dt, kind="ExternalOutput").ap()

    x_b = din("x_b", [S, D])
    x_own = din("x_own", [OWN, D])
    c_col = din("c_col", [128, 8])
    w_ada = din("w_ada", [2, D, 6 * D])
    b_ada = din("b_ada", [2, 6 * D])
    norm_mix = din("norm_mix", [2, D])
    norm_ffn = din("norm_ffn", [2, D])
    relb = din("relb", [32, 3, 8])
    ohc = din("ohc", [32, 3, 384])
    a_w_in = din("a_w_in", [D, 2, 3, 768])
    a_w_out = din("a_w_out", [D, D])
    a_gq = din("a_gq", [128, 1])
    a_gk = din("a_gk", [128, 1])
    b_w_in = din("b_w_in", [D, 1544])
    b_fb = din("b_fb", [1, 8])
    b_w_out = din("b_w_out", [D, D])
    b_gq = din("b_gq", [128, 1])
    b_gk = din("b_gk", [128, 1])
    r_w = din("r_w", [2, D, 20])
    r_b = din("r_b", [2, 20])
    small = stop in ("M", "A1", "A", "A2", "X1")
    w_gate = din("w_gate", [2, 16, D, 256] if not small else [1, 1, 8, 8])
    w_up = din("w_up", [2, 16, D, 256] if not small else [1, 1, 8, 8])
    w_down = din("w_down", [2, 16, 256, D] if not small else [1, 1, 8, 8])
    sel = din("sel", [128, 2])
    out = dout("out", [OWN, D])

    modrow = nc.dram_tensor("modrow", [2, 6 * D], F32)
    mscr = nc.dram_tensor("mscr", [3, 8, 128, 384], F32)
    sendA = [nc.dram_tensor(f"sendA{c}", [128, S], BF16) for c in range(4)]
    recvA = [nc.dram_tensor(f"recvA{c}", [256, S], BF16) for c in range(4)]
    dbg = {}
    if stop == "A":
        dbg["oA"] = dout("dbg_oA", [512, S], BF16)
    if stop == "M":
        dbg["mod"] = dout("dbg_mod", [2, 6 * D])
        dbg["mscr"] = dout("dbg_mscr", [3, 8, 128, 384])
    if stop == "A1":
        dbg["hmT"] = dout("dbg_hmT", [128, 8, S], BF16)
    if stop == "A2":
        dbg["qkn"] = dout("dbg_qkn", [128, 512], BF16)
        dbg["QT"] = dout("dbg_QT", [2, 128, 2, 128], BF16)
        dbg["KT"] = dout("dbg_KT", [3, 128, 2, 128], BF16)
        dbg["Vp"] = dout("dbg_Vp", [3, 128, 4, 128], BF16)
        dbg["PT"] = dout("dbg_PT", [128, 4, 256], BF16)
        dbg["acc"] = dout("dbg_acc", [128, 4, S], F32)
        dbg["oA"] = dout("dbg_oA", [512, S], BF16)

    P = Prog(nc)
    b_modrow = P.buf("modrow")
    b_mscr = P.buf("mscr")
    b_sendA = [P.buf(f"sendA{c}") for c in range(4)]
    b_recvA = [P.buf(f"recvA{c}") for c in range(4)]
    b_out = P.buf("out")

    with Phase(P, "M") as ph:
        ccol, b_ccol = ph.sb("ccol", [128, 8], F32)
        scol, b_scol = ph.sb("scol", [128, 8], F32)
        wst = [ph.sb("wst", [128, 8, 1024], F32) for _ in range(3)]
        badat2 = [ph.sb("bada", [1, 512], F32) for _ in range(2)]
        rowt = [ph.sb("rowt", [1, 512], F32) for _ in range(2)]
        pM, b_pM = ph.ps("pM", [128, 512])
        P.dma("sp", ccol[:], c_col, writes=[b_ccol])
        P.op("act", lambda e: e.activation(scol[:], ccol[:], AF.Silu), reads=[b_ccol], writes=[b_scol])
        it = 0
        for l in range(2):
            for cg2 in range(6):
                w_t, b_w = wst[it % 3]
                P.dma("sp" if it % 2 == 0 else "pool", w_t[:],
                      w_ada[l, :, cg2 * 1024:(cg2 + 1) * 1024].rearrange("(kc p) n -> p kc n", p=128), writes=[b_w])
                for h2 in range(2):
                    cg = cg2 * 2 + h2
                    r_t, b_r = rowt[cg % 2]
                    badat, b_badat = badat2[cg % 2]
                    P.dma("sp", badat[:], b_ada[l:l + 1, cg * 512:(cg + 1) * 512], writes=[b_badat])
                    for kc in range(8):
                        P.op("pe", lambda e, kc=kc, w_t=w_t, h2=h2: e.matmul(pM[0:1, :], scol[:, kc:kc + 1], w_t[:, kc, h2 * 512:(h2 + 1) * 512],
                                                                       start=(kc == 0), stop=(kc == 7)),
                             reads=[b_scol, b_w], writes=[b_pM], inc=(kc == 7))
                    P.op("dve", lambda e, r_t=r_t, badat=badat: e.tensor_tensor(r_t[:], pM[0:1, :], badat[:], ALU.add),
                         reads=[b_pM, b_badat], writes=[b_r])
                    P.dma("sp", modrow.ap()[l:l + 1, cg * 512:(cg + 1) * 512], r_t[:], reads=[b_r], writes=[b_modrow])
                it += 1
        relt, b_relt = ph.sb("relt", [32, 3, 8], F32)
        oht, b_oht = ph.sb("oht", [32, 3, 384], F32)
        bvec, b_bvec = ph.sb("bvec", [8, 3, 384], F32)
        P.dma("sp", relt[:], relb, writes=[b_relt])
        P.dma("sp", oht[:], ohc, writes=[b_oht])
        for g in range(3):
            P.op("pe", lambda e, g=g: e.matmul(pM[0:8, 0:384], relt[:, g, :], oht[:, g, :], start=True, stop=True),
                 reads=[b_relt, b_oht], writes=[b_pM])
            P.op("act", lambda e, g=g: e.activation(bvec[:, g, :], pM[0:8, 0:384], AF.Exp),
                 reads=[b_pM], writes=[b_bvec])
        P.op("dve", lambda e: e.memset(bvec[:, :, 0:127], 0.0), writes=[b_bvec])
        P.op("dve", lambda e: e.memset(bvec[:, :, 256:384], 0.0), writes=[b_bvec])
        for g in range(3):
            P.dma("sp", mscr.ap()[g], bvec[:, g, :].unsqueeze(1).to_broadcast([8, 128, 384]),
                  reads=[b_bvec], writes=[b_mscr])

    if stop == "M":
        P.dma("sp", dbg["mod"], modrow.ap(), reads=[b_modrow], writes=[b_out])
        P.dma("sp", dbg["mscr"], mscr.ap(), reads=[b_mscr], writes=[b_out])
        P.barrier()
        P.flush()
        P.stack.close()
        return nc

    with Phase(P, "A") as ph:
        hmT, _ = ph.sb("hmT", [128, 8, S], BF16)
        b_hmT = [P.buf(f"hmT{t}") for t in range(32)]
        acc, b_acc = ph.sb("acc", [128, 4, S], F32)
        wA = [ph.sb("wA", [128, 8, 768], BF16) for _ in range(1)]
        wstg = [ph.sb("wstg", [128, 768], F32) for _ in range(2)]
        Et, b_E = ph.sb("E", [128, 3, 4, 256], F32)
        xt = [ph.sb("xt", [128, D], F32) for _ in range(2)]
        tmod2 = [ph.sb("tmod", [128, D], F32) for _ in range(2)]
        tmod, b_tmod = tmod2[0]
        hmb2 = [ph.sb("hmb", [128, D], BF16) for _ in range(2)]
        hmb, b_hmb = hmb2[0]
        Abc, b_Abc = ph.sb("Abc", [128, D], F32)
        Sbc, b_Sbc = ph.sb("Sbc", [128, D], F32)
        ssx, b_ssx = ph.sb("ssx", [128, 32], F32)
        rsx, b_rsx = ph.sb("rsx", [128, 32], F32)
        ident, b_ident = ph.sb("ident", [128, 128], BF16)
        onesf, b_onesf = ph.sb("onesf", [128, 64], F32)
        gq, b_gq_ = ph.sb("gq", [128, 1], F32)
        gk, b_gk_ = ph.sb("gk", [128, 1], F32)
        sq, b_sq = ph.sb("sq", [128, 512], F32)
        rec, b_rec = sq, b_sq
        ssq, b_ssq = ph.sb("ssq", [128, 8], F32)
        rsq, b_rsq = ph.sb("rsq", [128, 8], F32)
        qkn2 = [ph.sb("qkn", [128, 512], BF16) for _ in range(2)]
        qkn, b_qkn = qkn2[0]
        QT = [ph.sb("QT", [128, 2, 128], BF16) for _ in range(2)]
        KT = [ph.sb("KT", [128, 2, 128], BF16) for _ in range(3)]
        Vp = [ph.sb("Vp", [128, 4, 128], BF16) for _ in range(4)]
        ex, b_ex = ph.sb("ex", [128, 4, 256], F32)
        PT, b_PT = ph.sb("PT", [128, 4, 256], BF16)
        oTc = [ph.sb("oTc", [64, 512], BF16) for _ in range(2)]
        pPs = [ph.ps("pP", [128, 1024]) for _ in range(2)]
        b_pP1s = [P.buf("pP1", excl=True) for _ in range(2)]
        pT, b_pT = ph.ps("pT", [128, 8, 128], BF16)
        pS, b_pS = ph.ps("pS", [128, 4, 256])
        pO, b_pO = ph.ps("pO", [128, 4, 128])
        pB, b_pB = pO[:].rearrange("p h q -> p (h q)"), b_pO

        P.op("pool", lambda e: e.memset(ident[:], 1.0), writes=[b_ident])
        P.op("pool", lambda e: e.affine_select(ident[:], ident[:], [[-1, 128]], ALU.is_equal, 0.0,
                                               base=0, channel_multiplier=1), reads=[b_ident], writes=[b_ident])
        P.op("pool", lambda e: e.memset(onesf[:], 1.0), writes=[b_onesf])
        for i in range(4):
            P.op("pool", lambda e, i=i: e.memset(Vp[i][0][:], 1.0), writes=[Vp[i][1]])
        P.op("dve", lambda e: e.memset(ssx[:], 0.0), writes=[b_ssx])
        if stop == "A2":
            for i in range(2):
                P.op("pool", lambda e, i=i: e.memset(QT[i][0][:], 0.0), writes=[QT[i][1]])
            for i in range(3):
                P.op("pool", lambda e, i=i: e.memset(KT[i][0][:], 0.0), writes=[KT[i][1]])
            P.op("pool", lambda e: e.memset(PT[:], 0.0), writes=[b_PT])
            P.op("pool", lambda e: e.memset(acc[:], 0.0), writes=[b_acc])
        P.dma("sp", gq[:], a_gq, writes=[b_gq_])
        P.dma("sp", gk[:], a_gk, writes=[b_gk_])
        mr = modrow.ap()
        P.dma("sp", Sbc[:], mr[0:1, 0:D].to_broadcast([128, D]), reads=[b_modrow], writes=[b_Sbc])
        P.dma("sp", Abc[:], mr[0:1, D:2 * D].to_broadcast([128, D]), reads=[b_modrow], writes=[b_Abc])
        P.dma("sp", tmod[:], norm_mix[0:1, :].to_broadcast([128, D]), writes=[b_tmod])
        P.op("dve", lambda e: e.scalar_tensor_tensor(Abc[:], Abc[:], 1.0, tmod[:], ALU.add, ALU.mult),
             reads=[b_Abc, b_tmod], writes=[b_Abc])

        def a1_pre(tt):
            x_t, b_x = xt[tt % 2]
            tm_t, b_tm = tmod2[tt % 2]
            hb_t, b_hb = hmb2[tt % 2]
            P.dma("sp", x_t[:], x_b[tt * 128:(tt + 1) * 128, :], writes=[b_x])
            P.op("act", lambda e, x_t=x_t, tt=tt: e.activation(ex[:].rearrange("p h w -> p (h w)"), x_t[:], AF.Square, accum_out=ssx[:, tt:tt + 1]),
                 reads=[b_x], writes=[b_ex, b_ssx])
            P.op("act", lambda e, tt=tt: e.activation(rsx[:, tt:tt + 1], ssx[:, tt:tt + 1], AF.Ln, bias=EPS, scale=1.0 / D),
                 reads=[b_ssx], writes=[b_rsx])
            P.op("act", lambda e, tt=tt: e.activation(rsx[:, tt:tt + 1], rsx[:, tt:tt + 1], AF.Exp, scale=-0.5), reads=[b_rsx], writes=[b_rsx])
            P.op("dve", lambda e, x_t=x_t, tt=tt, tm_t=tm_t: e.scalar_tensor_tensor(tm_t[:], x_t[:], rsx[:, tt:tt + 1], Abc[:], ALU.mult, ALU.mult),
                 reads=[b_x, b_rsx, b_Abc], writes=[b_tm])
            P.op("pool", lambda e, tm_t=tm_t, hb_t=hb_t: e.tensor_tensor(hb_t[:], tm_t[:], Sbc[:], ALU.add),
                 reads=[b_tm, b_Sbc], writes=[b_hb])

        def a1_pe(tt):
            hb_t, b_hb = hmb2[tt % 2]
            for kc in range(8):
                P.op("pe", lambda e, kc=kc, hb_t=hb_t: e.transpose(pT[:, kc, :], hb_t[:, kc * 128:(kc + 1) * 128], ident[:]),
                     reads=[b_hb, b_ident], writes=[b_pT], inc=(kc == 7))

        def a1_back(tt):
            P.op("act", lambda e, tt=tt: e.activation(hmT[:, :, tt * 128:(tt + 1) * 128], pT[:], AF.Copy),
                 reads=[b_pT], writes=[b_hmT[tt]])

        a1_pre(0)
        a1_pe(0)
        for tt in range(32):
            if tt + 1 < 32:
                a1_pre(tt + 1)
            a1_back(tt)
            if tt + 1 < 32:
                a1_pe(tt + 1)
        if stop == "A1":
            P.dma("sp", dbg["hmT"], hmT[:], reads=b_hmT, writes=[b_out])
        stg = 0
        wsel = 0
        for p in range(2 if stop != "A1" else 0):
            for g in range(3):
                for h in range(4):
                    src = bass.AP(mscr, ((g * 8 + 4 * p + h) * 128) * 384 + 127, [[383, 128], [1, 256]])
                    P.dma("sp", Et[:, g, (h % 2) * 2 + h // 2, :], src, reads=[b_mscr], writes=[b_E])
            for g in range(3):
                r = DIL[g]
                w_t, b_w = wA[0]
                wsel += 1
                for kc in range(8):
                    s_t, b_s = wstg[stg % 2]
                    stg += 1
                    P.dma("sp", s_t[:], a_w_in[kc * 128:(kc + 1) * 128, p, g, :], writes=[b_s])
                    P.op("pool", lambda e, s_t=s_t, w_t=w_t, kc=kc: e.tensor_copy(w_t[:, kc, :], s_t[:]),
                         reads=[b_s], writes=[b_w])
                nblk = S // r // 128
                steps = [(j, nb) for j in range(r) for nb in range(nblk)]
                if nsteps is not None:
                    steps = steps[:nsteps] if (p == 0 and g == 0) else []

                def s1a(si, j, nb, part=None, r=r, w_t=w_t, b_w=b_w):
                    start = 128 * nb * r + j
                    stop_ = start + 127 * r + 1
                    tl0, tl1 = start // 128, (stop_ - 1) // 128
                    hb = b_hmT[tl0:tl1 + 1]
                    qn_t, b_qn = qkn2[si % 2]
                    pP, b_pP = pPs[si % 2]
                    b_pP1 = b_pP1s[si % 2]
                    kcs = range(8) if part is None else (range(0, 4) if part == 0 else range(4, 8))
                    for kc in kcs:
                        lhs = hmT[:, kc, start:stop_:r]
                        P.op("pe", lambda e, lhs=lhs, kc=kc, pP=pP: e.matmul(pP[:, 0:512], lhs, w_t[:, kc, 0:512],
                                                                      start=(kc == 0), stop=(kc == 7)),
                             reads=hb + [b_w], writes=[b_pP], inc=False)
                        P.op("pe", lambda e, lhs=lhs, kc=kc, pP=pP: e.matmul(pP[:, 512:768], lhs, w_t[:, kc, 512:768],
                                                                      start=(kc == 0), stop=(kc == 7)),
                             reads=hb + [b_w], writes=[b_pP1], inc=(kc == 7))
                    if part == 0:
                        return
                    P.op("act", lambda e, pP=pP: e.activation(sq[:], pP[:, 0:512], AF.Square), reads=[b_pP], writes=[b_sq])
                    v_t, b_v = Vp[si % 4]
                    P.op("act", lambda e, v_t=v_t, pP=pP: e.activation(v_t[:, :, 0:64], pP[:, 512:768].rearrange("p (h d) -> p h d", d=64), AF.Copy),
                         reads=[b_pP1], writes=[b_v])
                    P.op("dve", lambda e: e.tensor_reduce(ssq[:], sq[:].rearrange("p (h d) -> p h d", d=64), AX.X, ALU.add),
                         reads=[b_sq], writes=[b_ssq])
                    P.op("act", lambda e: e.activation(rsq[:], ssq[:], AF.Ln, bias=EPS, scale=1.0 / 64),
                         reads=[b_ssq], writes=[b_rsq])
                    P.op("act", lambda e: e.activation(rsq[:], rsq[:], AF.Exp, scale=-0.5), reads=[b_rsq], writes=[b_rsq])
                    P.op("dve", lambda e, qn_t=qn_t, pP=pP: e.tensor_tensor(qn_t[:].rearrange("p (h d) -> p h d", d=64),
                                                          pP[:, 0:512].rearrange("p (h d) -> p h d", d=64),
                                                          rsq[:].unsqueeze(2).to_broadcast([128, 8, 64]), ALU.mult),
                         reads=[b_pP, b_rsq], writes=[b_qn])

                def s1b(si, j, nb):
                    qn_t, b_qn = qkn2[si % 2]
                    for c4 in range(4):
                        P.op("pe", lambda e, c4=c4, qn_t=qn_t: e.transpose(pT[:, c4, :], qn_t[:, c4 * 128:(c4 + 1) * 128], ident[:]),
                             reads=[b_qn, b_ident], writes=[b_pT], inc=(c4 == 3))
                    q_t, b_q = QT[si % 2]
                    k_t, b_k = KT[si % 3]
                    P.op("act", lambda e, q_t=q_t: e.activation(q_t[:], pT[:, 0:2, :], AF.Copy, scale=gq[:, 0:1]),
                         reads=[b_pT, b_gq_], writes=[b_q])
                    P.op("dve", lambda e, k_t=k_t: e.tensor_scalar(k_t[:], pT[:, 2:4, :], gk[:, 0:1], None, ALU.mult),
                         reads=[b_pT, b_gk_], writes=[b_k])

                def s2a(si, j, nb, r=r, g=g, p=p):
                    q_t, b_q = QT[si % 2]
                    k_t, b_k = KT[si % 3]
                    kp_t, b_kp = KT[(si - 1) % 3]
                    W = 128 if nb == 0 else 256
                    last = 3
                    for h in range(4):
                        pr, hp = h // 2, h % 2
                        lo, hi = 64 * hp, 64 * hp + 64
                        hq = hp * 2 + pr
                        P.op("pe", lambda e, hq=hq, pr=pr, lo=lo, hi=hi: e.matmul(pS[:, hq, 0:128], k_t[lo:hi, pr, :], q_t[lo:hi, pr, :],
                                                                               start=True, stop=True),
                             reads=[b_k, b_q], writes=[b_pS], inc=(nb == 0 and h == last))
                        if nb > 0:
                            P.op("pe", lambda e, hq=hq, pr=pr, lo=lo, hi=hi: e.matmul(pS[:, hq, 128:256], kp_t[lo:hi, pr, :], q_t[lo:hi, pr, :],
                                                                                   start=True, stop=True),
                                 reads=[b_kp, b_q], writes=[b_pS], inc=(h == last))
                    P.op("act", lambda e, W=W: e.activation(ex[:, :, 0:W], pS[:, :, 0:W], AF.Exp, scale=0.125),
                         reads=[b_pS], writes=[b_ex])
                    P.op("dve", lambda e, W=W, g=g: e.tensor_tensor(PT[:, :, 0:W], ex[:, :, 0:W], Et[:, g, :, 0:W], ALU.mult),
                         reads=[b_ex, b_E], writes=[b_PT])

                def s2b(si, j, nb, r=r, g=g, p=p):
                    start = 128 * nb * r + j
                    stop_ = start + 127 * r + 1
                    v_t, b_v = Vp[si % 4]
                    vp_t, b_vp = Vp[(si - 1) % 4]
                    last = 3
                    for h in range(4):
                        hq = (h % 2) * 2 + h // 2
                        P.op("pe", lambda e, h=h, hq=hq: e.matmul(pO[:, h, :], v_t[:, h, :], PT[:, hq, 0:128],
                                                           start=True, stop=(nb == 0)),
                             reads=[b_v, b_PT], writes=[b_pO], inc=(nb == 0 and h == last))
                        if nb > 0:
                            P.op("pe", lambda e, h=h, hq=hq: e.matmul(pO[:, h, :], vp_t[:, h, :], PT[:, hq, 128:256],
                                                               start=False, stop=True),
                                 reads=[b_vp, b_PT], writes=[b_pO], inc=(h == last))
                    av = acc[:, :, start:stop_:r]
                    if g == 0:
                        P.op("act", lambda e, av=av: e.activation(av, pO[:], AF.Copy), reads=[b_pO], writes=[b_acc])
                    else:
                        P.op("dve", lambda e, av=av: e.tensor_tensor(av, av, pO[:], ALU.add),
                             reads=[b_pO, b_acc], writes=[b_acc])

                n_ = len(steps)
                if n_ > 0:
                    s1a(0, *steps[0])
                if n_ > 1:
                    s1a(1, *steps[1])
                if n_ > 0:
                    s1b(0, *steps[0])
                for si in range(n_):
                    if do2:
                        s2a(si, *steps[si])
                    if si + 2 < n_:
                        s1a(si + 2, *steps[si + 2], part=0)
                    if si + 1 < n_:
                        s1b(si + 1, *steps[si + 1])
                    if si + 2 < n_:
                        s1a(si + 2, *steps[si + 2], part=1)
                    if do2:
                        s2b(si, *steps[si])

            k = 0
            recA = [(sq, b_sq), (ex[:].rearrange("p h w -> p (h w)"), b_ex)]
            pBA = [(pO[:].rearrange("p h q -> p (h q)"), b_pO), (pS[:].rearrange("p h w -> p (h w)"), b_pS)]
            for h in range(4 if donorm else 0):
                for tg in range(8):
                    ts_ = slice(tg * 512, (tg + 1) * 512)
                    o_t, b_o = oTc[k % 2]
                    rec, b_rec = recA[k % 2]
                    pB, b_pB = pBA[k % 2]
                    k += 1
                    P.op("act", lambda e, h=h, ts_=ts_, rec=rec: e.activation(rec[64:65, 0:512], acc[64:65, h, ts_], AF.Ln),
                         reads=[b_acc], writes=[b_rec])
                    P.op("act", lambda e, rec=rec: e.activation(rec[64:65, 0:512], rec[64:65, 0:512], AF.Exp, scale=-1.0),
                         reads=[b_rec], writes=[b_rec])
                    P.op("pe", lambda e, rec=rec, pB=pB: e.matmul(pB[0:64, 0:512], onesf[64:65, 0:64], rec[64:65, 0:512], start=True, stop=True),
                         reads=[b_onesf, b_rec], writes=[b_pB])
                    P.op("dve", lambda e, h=h, ts_=ts_, o_t=o_t, pB=pB: e.tensor_tensor(o_t[:], acc[0:64, h, ts_], pB[0:64, 0:512], ALU.mult),
                         reads=[b_acc, b_pB], writes=[b_o])
                    slot = 4 * p + h
                    row = (slot % 2) * 64
                    P.dma("sp", sendA[slot // 2].ap()[row:row + 64, ts_], o_t[:], reads=[b_o], writes=[b_sendA[slot // 2]])
        if stop == "A2":
            P.dma("sp", dbg["qkn"], qkn[:], reads=[b_qkn], writes=[b_out])
            for i in range(2):
                P.dma("sp", dbg["QT"][i], QT[i][0][:], reads=[QT[i][1]], writes=[b_out])
            for i in range(3):
                P.dma("sp", dbg["KT"][i], KT[i][0][:], reads=[KT[i][1]], writes=[b_out])
                P.dma("sp", dbg["Vp"][i], Vp[i][0][:], reads=[Vp[i][1]], writes=[b_out])
            P.dma("sp", dbg["PT"], PT[:], reads=[b_PT], writes=[b_out])
            P.dma("sp", dbg["acc"], acc[:], reads=[b_acc], writes=[b_out])

    if stop == "A1":
        P.stack.close()
        return nc
    if stop in ("A", "A2"):
        for c in range(4):
            P.dma("sp", dbg["oA"][c * 128:(c + 1) * 128, :], sendA[c].ap(), reads=[b_sendA[c]], writes=[b_out])
        P.barrier()
        P.flush()
        P.stack.close()
        return nc

    sendH = [nc.dram_tensor(f"sendH{c}", [256, OWN], BF16) for c in range(4)]
    recvH = [nc.dram_tensor(f"recvH{c}", [512, OWN], BF16) for c in range(4)]
    xmid = nc.dram_tensor("xmid", [OWN, D], F32)
    sendC = [nc.dram_tensor(f"sendC{c}", [128, S], BF16) for c in range(4)]
    recvC = [nc.dram_tensor(f"recvC{c}", [256, S], BF16) for c in range(4)]
    b_sendH = [P.buf(f"sendH{c}") for c in range(4)]; b_recvH = [P.buf(f"recvH{c}") for c in range(4)]; b_xmid = P.buf("xmid")
    b_sendC = [P.buf(f"sendC{c}") for c in range(4)]; b_recvC = [P.buf(f"recvC{c}") for c in range(4)]
    mr = modrow.ap()

    def phase_B(l, send_t, b_send, recv_t, b_recv, w_out_ap, stopB=None):
        with Phase(P, f"B{l}") as po:
            xres, _ = po.sb("xres", [128, 16, D], F32)
            b_xr = [P.buf(f"xres{t}") for t in range(16)]
            hfT, b_hfT = po.sb("hfT", [128, 8, OWN], BF16)
            gT, b_gT = po.sb("gT", [16, OWN], BF16)
            with Phase(P, f"B1{l}") as ph:
                selt, b_selt = ph.sb("selt", [128, 2], F32)
                Gbc, b_Gbc = ph.sb("Gbc", [128, D], F32)
                woB, b_woB = ph.sb("woB", [128, 8, D], BF16)
                wst = [ph.sb("wst", [128, D], F32) for _ in range(2)]
                cand = [[ph.sb("cand", [128, 8, 512], BF16) for _ in range(2)] for _ in range(2)]
                otmp, b_otmp = ph.sb("otmp", [128, 8, 512], BF16)
                osel = [ph.sb("osel", [128, 8, 512], BF16) for _ in range(2)]
                pY = [ph.ps("pY", [128, 1024]) for _ in range(2)]
                P.dma("sp", selt[:], sel, writes=[b_selt])
                P.dma("sp", Gbc[:], mr[l:l + 1, 2 * D:3 * D].to_broadcast([128, D]), reads=[b_modrow], writes=[b_Gbc])
                xsrc = x_own if l == 0 else xmid.ap()
                for t4 in range(4):
                    for t in range(4 * t4, 4 * t4 + 4):
                        P.dma("sp", xres[:, t, :], xsrc[t * 128:(t + 1) * 128, :],
                              reads=([b_xmid] if l == 1 else []), writes=[b_xr[t]])
                for kc in range(8):
                    w_t, b_w = wst[kc % 2]
                    P.dma("sp", w_t[:], w_out_ap[kc * 128:(kc + 1) * 128, :], writes=[b_w])
                    P.op("dve", lambda e, w_t=w_t, kc=kc: e.tensor_tensor(woB[:, kc, :], w_t[:], Gbc[:], ALU.mult),
                         reads=[b_w, b_Gbc], writes=[b_woB])
                for c in range(4):
                    P.collective("AllGather", send_t[c], recv_t[c], reads=[b_send[c]], writes=[b_recv[c]])
                for tg in range(4):
                    c0_, c1_ = cand[tg % 2]
                    o_t, b_o = osel[tg % 2]
                    for rk, (c_t, b_c) in enumerate((c0_, c1_)):
                        off = rk * OWN + tg * 512
                        for kc in range(8):
                            P.dma("sp", c_t[:, kc, :], recv_t[kc % 4].ap()[(kc // 4) * 128:(kc // 4 + 1) * 128, off:off + 512],
                                  reads=[b_recv[kc % 4]], writes=[b_c])
                    P.op("dve", lambda e, c_t=c0_[0]: e.tensor_scalar(otmp[:], c_t[:], selt[:, 0:1], None, ALU.mult),
                         reads=[c0_[1], b_selt], writes=[b_otmp])
                    P.op("dve", lambda e, c_t=c1_[0], o_t=o_t: e.scalar_tensor_tensor(o_t[:], c_t[:], selt[:, 1:2], otmp[:], ALU.mult, ALU.add),
                         reads=[c1_[1], b_selt, b_otmp], writes=[b_o])
                    for t4 in range(4):
                        T = tg * 4 + t4
                        y_t, b_y = pY[T % 2]
                        for half in range(2):
                            for kc in range(8):
                                P.op("pe", lambda e, o_t=o_t, kc=kc, t4=t4, half=half, y_t=y_t: e.matmul(
                                    y_t[:, half * 512:(half + 1) * 512], o_t[:, kc, t4 * 128:(t4 + 1) * 128],
                                    woB[:, kc, half * 512:(half + 1) * 512], start=(kc == 0), stop=(kc == 7)),
                                    reads=[b_o, b_woB], writes=[b_y], inc=(kc == 7 and half == 1))
                        P.op("dve", lambda e, T=T, y_t=y_t: e.tensor_tensor(xres[:, T, :], xres[:, T, :], y_t[:], ALU.add),
                             reads=[b_y, b_xr[T]], writes=[b_xr[T]])
            if stopB == "B1":
                P.dma("sp", dbg["x"], xres[:], reads=b_xr, writes=[b_out])
                return
            NS = 5
            Gf, b_Gf = po.sb("Gf", [128, D], F32)
            wg = [po.sb("wg", [128, 8, 256], BF16) for _ in range(NS)]
            wu = [po.sb("wu", [128, 8, 256], BF16) for _ in range(NS)]
            wd = [po.sb("wd", [128, 2, D], BF16) for _ in range(NS)]
            stg = [po.sb("stg", [128, 2, D], F32) for _ in range(1)]
            P.dma("sp", Gf[:], mr[l:l + 1, 5 * D:6 * D].to_broadcast([128, D]), reads=[b_modrow], writes=[b_Gf])
            sc_ = [0]

            def load_expert(e_):
                s_ = e_ % NS
                P.dma("pool", wg[s_][0][:], w_gate[l, e_].rearrange("(kc p) f -> p kc f", p=128), writes=[wg[s_][1]])
                P.dma("pool", wu[s_][0][:], w_up[l, e_].rearrange("(kc p) f -> p kc f", p=128), writes=[wu[s_][1]])
                st_t, b_st = stg[0]
                sc_[0] += 1
                P.dma("sp", st_t[:], w_down[l, e_].rearrange("(fc p) d -> p fc d", p=128), writes=[b_st])
                P.op("dve", lambda e, st_t=st_t, s_=s_: e.tensor_tensor(wd[s_][0][:], st_t[:], Gf[:].unsqueeze(1).to_broadcast([128, 2, D]), ALU.mult),
                     reads=[b_st, b_Gf], writes=[wd[s_][1]])

            with Phase(P, f"B2{l}") as ph:
                Abc, b_Abc = ph.sb("Abc", [128, D], F32)
                Sbc, b_Sbc = ph.sb("Sbc", [128, D], F32)
                tmod, b_tmod = ph.sb("tmod", [128, D], F32)
                junk, b_junk = ph.sb("junk", [128, D], BF16)
                hf32 = [ph.sb("hf32", [128, D], F32) for _ in range(1)]
                hfT32 = [ph.sb("hfT32", [128, 8, 128], F32) for _ in range(1)]
                identf, b_identf = ph.sb("identf", [128, 128], F32)
                identb, b_identb = ph.sb("identb", [128, 128], BF16)
                ssx, b_ssx = ph.sb("ssx", [128, 16], F32)
                rsx, b_rsx = ph.sb("rsx", [128, 16], F32)
                rw, b_rw = ph.sb("rw", [128, 8, 20], F32)
                rbb, b_rbb = ph.sb("rbb", [128, 20], F32)
                lg, b_lg = ph.sb("lg", [128, 16, 20], F32)
                pXf = [ph.ps("pXf", [128, 8, 128]) for _ in range(2)]
                pR, b_pR = ph.ps("pR", [128, 512])
                pGT, b_pGT = ph.ps("pGT", [128, 1024], BF16)
                P.op("pool", lambda e: e.memset(identf[:], 1.0), writes=[b_identf])
                P.op("pool", lambda e: e.affine_select(identf[:], identf[:], [[-1, 128]], ALU.is_equal, 0.0,
                                                       base=0, channel_multiplier=1), reads=[b_identf], writes=[b_identf])
                P.op("pool", lambda e: e.tensor_copy(identb[:], identf[:]), reads=[b_identf], writes=[b_identb])
                P.op("dve", lambda e: e.memset(ssx[:], 0.0), writes=[b_ssx])
                P.dma("sp", Sbc[:], mr[l:l + 1, 3 * D:4 * D].to_broadcast([128, D]), reads=[b_modrow], writes=[b_Sbc])
                P.dma("sp", Abc[:], mr[l:l + 1, 4 * D:5 * D].to_broadcast([128, D]), reads=[b_modrow], writes=[b_Abc])
                P.dma("sp", tmod[:], norm_ffn[l:l + 1, :].to_broadcast([128, D]), writes=[b_tmod])
                P.op("dve", lambda e: e.scalar_tensor_tensor(Abc[:], Abc[:], 1.0, tmod[:], ALU.add, ALU.mult),
                     reads=[b_Abc, b_tmod], writes=[b_Abc])
                P.dma("sp", rw[:], r_w[l].rearrange("(kc p) n -> p kc n", p=128), writes=[b_rw])
                P.dma("sp", rbb[:], r_b[l:l + 1, :].to_broadcast([128, 20]), writes=[b_rbb])
                def b2_pre(T):
                    h_t, b_h = hf32[0]
                    P.op("act", lambda e, T=T: e.activation(junk[:], xres[:, T, :], AF.Square, accum_out=ssx[:, T:T + 1]),
                         reads=[b_xr[T]], writes=[b_junk, b_ssx])
                    P.op("act", lambda e, T=T: e.activation(rsx[:, T:T + 1], ssx[:, T:T + 1], AF.Ln, bias=EPS, scale=1.0 / D),
                         reads=[b_ssx], writes=[b_rsx])
                    P.op("act", lambda e, T=T: e.activation(rsx[:, T:T + 1], rsx[:, T:T + 1], AF.Exp, scale=-0.5), reads=[b_rsx], writes=[b_rsx])
                    P.op("dve", lambda e, T=T: e.scalar_tensor_tensor(tmod[:], xres[:, T, :], rsx[:, T:T + 1], Abc[:], ALU.mult, ALU.mult),
                         reads=[b_xr[T], b_rsx, b_Abc], writes=[b_tmod])
                    P.op("pool", lambda e, h_t=h_t: e.tensor_tensor(h_t[:], tmod[:], Sbc[:], ALU.add),
                         reads=[b_tmod, b_Sbc], writes=[b_h])

                def b2_pe1(T):
                    h_t, b_h = hf32[0]
                    x_t, b_xT = pXf[T % 2]
                    for kc in range(8):
                        P.op("pe", lambda e, kc=kc, h_t=h_t, x_t=x_t: e.transpose(x_t[:, kc, :], h_t[:, kc * 128:(kc + 1) * 128], identf[:]),
                             reads=[b_h, b_identf], writes=[b_xT], inc=(kc == 7))

                def b2_back(T):
                    x_t, b_xT = pXf[T % 2]
                    f_t, b_f = hfT32[0]
                    P.op("act", lambda e, T=T, x_t=x_t: e.activation(hfT[:, :, T * 128:(T + 1) * 128], x_t[:], AF.Copy),
                         reads=[b_xT], writes=[b_hfT])
                    P.op("dve", lambda e, x_t=x_t, f_t=f_t: e.tensor_copy(f_t[:], x_t[:]), reads=[b_xT], writes=[b_f])
                    for kc in range(8):
                        P.op("pe", lambda e, kc=kc, f_t=f_t: e.matmul(pR[:, 0:20], f_t[:, kc, :], rw[:, kc, :], start=(kc == 0), stop=(kc == 7)),
                             reads=[b_f, b_rw], writes=[b_pR], inc=(kc == 7))
                    P.op("dve", lambda e, T=T: e.tensor_tensor(lg[:, T, :], pR[:, 0:20], rbb[:], ALU.add),
                         reads=[b_pR, b_rbb], writes=[b_lg])

                b2_pre(0)
                b2_pe1(0)
                pre_e = [0]
                for T in range(16):
                    if T + 1 < 16:
                        b2_pre(T + 1)
                    b2_back(T)
                    if T + 1 < 16:
                        b2_pe1(T + 1)
                    if T % 3 == 1 and pre_e[0] < NS:
                        load_expert(pre_e[0])
                        pre_e[0] += 1
                while pre_e[0] < NS:
                    load_expert(pre_e[0])
                    pre_e[0] += 1
                def sbt(name, shape, dt=F32):
                    return ph.sb(name, shape, dt)
                gmax, b_gmax = sbt("gmax", [128, 16])
                gsh, b_gsh = sbt("gsh", [128, 16, 4])
                gsum, b_gsum = sbt("gsum", [128, 16])
                gw, b_gw = sbt("gw", [128, 16])
                ohg, b_ohg = sbt("ohg", [128, 16, 4])
                tmp4, b_tmp4 = sbt("tmp4", [128, 16, 4, 4])
                esel, b_esel = sbt("esel", [128, 16, 4])
                m1, b_m1 = sbt("m1", [128, 16])
                mk1, b_mk1 = sbt("mk1", [128, 16, 4])
                e2, b_e2 = sbt("e2", [128, 16, 4])
                m2, b_m2 = sbt("m2", [128, 16])
                mk2, b_mk2 = sbt("mk2", [128, 16, 4])
                tt_, b_tt = sbt("tt", [128, 16])
                w1, b_w1 = sbt("w1", [128, 16])
                w2, b_w2 = sbt("w2", [128, 16])
                ing, b_ing = sbt("ing", [128, 16, 4])
                ing2, b_ing2 = sbt("ing2", [128, 16, 4])
                gates, b_gates = sbt("gates", [128, 16, 4, 4], BF16)
                gl = lg[:, :, 0:4]
                el = lg[:, :, 4:20].rearrange("p t (g e) -> p t g e", e=4)

                def bc3(ap2):
                    return ap2.unsqueeze(2).to_broadcast([128, 16, 4])
                V = lambda f, r, w: P.op("dve", f, reads=r, writes=w)
                V(lambda e: e.tensor_reduce(gmax[:], gl, AX.X, ALU.max), [b_lg], [b_gmax])
                V(lambda e: e.tensor_tensor(gsh[:], gl, bc3(gmax[:]), ALU.subtract), [b_lg, b_gmax], [b_gsh])
                V(lambda e: e.tensor_tensor(ohg[:], gl, bc3(gmax[:]), ALU.is_equal), [b_lg, b_gmax], [b_ohg])
                P.op("act", lambda e: e.activation(gsh[:], gsh[:], AF.Exp), reads=[b_gsh], writes=[b_gsh])
                V(lambda e: e.tensor_reduce(gsum[:], gsh[:], AX.X, ALU.add), [b_gsh], [b_gsum])
                V(lambda e: e.reciprocal(gw[:], gsum[:]), [b_gsum], [b_gw])
                V(lambda e: e.tensor_tensor(tmp4[:], el, ohg[:].unsqueeze(3).to_broadcast([128, 16, 4, 4]), ALU.mult),
                  [b_lg, b_ohg], [b_tmp4])
                V(lambda e: e.tensor_reduce(esel[:], tmp4[:].rearrange("p t g e -> p t e g"), AX.X, ALU.add), [b_tmp4], [b_esel])
                V(lambda e: e.tensor_reduce(m1[:], esel[:], AX.X, ALU.max), [b_esel], [b_m1])
                V(lambda e: e.tensor_tensor(mk1[:], esel[:], bc3(m1[:]), ALU.is_equal), [b_esel, b_m1], [b_mk1])
                V(lambda e: e.scalar_tensor_tensor(e2[:], mk1[:], -1e30, esel[:], ALU.mult, ALU.add), [b_mk1, b_esel], [b_e2])
                V(lambda e: e.tensor_reduce(m2[:], e2[:], AX.X, ALU.max), [b_e2], [b_m2])
                V(lambda e: e.tensor_tensor(mk2[:], e2[:], bc3(m2[:]), ALU.is_equal), [b_e2, b_m2], [b_mk2])
                V(lambda e: e.tensor_tensor(tt_[:], m2[:], m1[:], ALU.subtract), [b_m1, b_m2], [b_tt])
                P.op("act", lambda e: e.activation(tt_[:], tt_[:], AF.Exp), reads=[b_tt], writes=[b_tt])
                V(lambda e: e.tensor_scalar(w1[:], tt_[:], 1.0, None, ALU.add), [b_tt], [b_w1])
                V(lambda e: e.reciprocal(w1[:], w1[:]), [b_w1], [b_w1])
                V(lambda e: e.tensor_tensor(w1[:], w1[:], gw[:], ALU.mult), [b_w1, b_gw], [b_w1])
                V(lambda e: e.tensor_tensor(w2[:], w1[:], tt_[:], ALU.mult), [b_w1, b_tt], [b_w2])
                V(lambda e: e.tensor_tensor(ing[:], mk1[:], bc3(w1[:]), ALU.mult), [b_mk1, b_w1], [b_ing])
                V(lambda e: e.tensor_tensor(ing2[:], mk2[:], bc3(w2[:]), ALU.mult), [b_mk2, b_w2], [b_ing2])
                V(lambda e: e.tensor_tensor(ing[:], ing[:], ing2[:], ALU.add), [b_ing, b_ing2], [b_ing])
                V(lambda e: e.tensor_tensor(gates[:], ohg[:].unsqueeze(3).to_broadcast([128, 16, 4, 4]),
                                            ing[:].unsqueeze(2).to_broadcast([128, 16, 4, 4]), ALU.mult),
                  [b_ohg, b_ing], [b_gates])
                for T in range(16):
                    P.op("pe", lambda e, T=T: e.transpose(pGT[0:16, (T % 8) * 128:(T % 8 + 1) * 128],
                                                          gates[:, T].rearrange("p g e -> p (g e)"), identb[:]),
                         reads=[b_gates, b_identb], writes=[b_pGT], inc=(T % 8 == 7))
                    if T % 8 == 7:
                        P.op("dve", lambda e, T=T: e.tensor_copy(gT[:, (T - 7) * 128:(T + 1) * 128], pGT[0:16, :]),
                             reads=[b_pGT], writes=[b_gT])
                if stopB == "B2":
                    P.dma("sp", dbg["hfT"], hfT[:], reads=[b_hfT], writes=[b_out])
                    P.dma("sp", dbg["gT"], gT[:], reads=[b_gT], writes=[b_out])
                    P.dma("sp", dbg["lg"], lg[:], reads=[b_lg], writes=[b_out])
            if stopB == "B2":
                return
            with Phase(P, f"B3{l}") as ph:
                selm, b_selm = ph.sb("selm", [16, 16, 128], BF16)
                hid = [ph.sb("hid", [128, 8, 512], BF16) for _ in range(2)]
                Gsb = [ph.sb("Gsb", [128, 512], F32) for _ in range(1)]
                sg = [ph.sb("sg", [128, 512], F32) for _ in range(1)]
                t32 = [ph.sb("t32", [128, 512], F32) for _ in range(1)]
                pGb, b_pGb = ph.ps("pGb", [128, 512])
                pGa = [ph.ps("pGa", [128, 512]) for _ in range(2)]
                pUp = [ph.ps("pUp", [128, 512]) for _ in range(2)]
                pY2 = [ph.ps("pY2", [128, 1024]) for _ in range(1)]
                P.op("pool", lambda e: e.memset(selm[:], 1.0), writes=[b_selm])
                P.op("pool", lambda e: e.affine_select(selm[:], selm[:], [[-1, 16], [0, 128]], ALU.is_equal, 0.0,
                                                       base=0, channel_multiplier=1), reads=[b_selm], writes=[b_selm])
                cnt2 = 0
                for grp in range(4):
                    for tg in range(4):
                        h_t, b_h = hid[(grp * 4 + tg) % 2]
                        for ei in range(4):
                            e_ = grp * 4 + ei
                            s_ = e_ % NS
                            G_t, b_G = Gsb[0]
                            P.op("pe", lambda e, e_=e_, tg=tg: e.matmul(pGb[:, :], selm[0:16, e_, :], gT[0:16, tg * 512:(tg + 1) * 512],
                                                                         start=True, stop=True),
                                 reads=[b_selm, b_gT], writes=[b_pGb])
                            P.op("act", lambda e, G_t=G_t: e.activation(G_t[:], pGb[:], AF.Copy), reads=[b_pGb], writes=[b_G])
                            for fc in range(2):
                                ga_t, b_ga = pGa[cnt2 % 2]
                                up_t, b_up = pUp[cnt2 % 2]
                                s_t, b_s = sg[0]
                                t_t, b_t = t32[0]
                                cnt2 += 1
                                for kc in range(8):
                                    P.op("pe", lambda e, s_=s_, kc=kc, fc=fc, tg=tg, ga_t=ga_t: e.matmul(
                                        ga_t[:], wg[s_][0][:, kc, fc * 128:(fc + 1) * 128], hfT[:, kc, tg * 512:(tg + 1) * 512],
                                        start=(kc == 0), stop=(kc == 7)),
                                        reads=[wg[s_][1], b_hfT], writes=[b_ga], inc=(kc == 7))
                                for kc in range(8):
                                    P.op("pe", lambda e, s_=s_, kc=kc, fc=fc, tg=tg, up_t=up_t: e.matmul(
                                        up_t[:], wu[s_][0][:, kc, fc * 128:(fc + 1) * 128], hfT[:, kc, tg * 512:(tg + 1) * 512],
                                        start=(kc == 0), stop=(kc == 7)),
                                        reads=[wu[s_][1], b_hfT], writes=[b_up], inc=(kc == 7))
                                P.op("act", lambda e, s_t=s_t, ga_t=ga_t: e.activation(s_t[:], ga_t[:], AF.Silu), reads=[b_ga], writes=[b_s])
                                P.op("dve", lambda e, s_t=s_t, up_t=up_t, t_t=t_t: e.tensor_tensor(t_t[:], s_t[:], up_t[:], ALU.mult),
                                     reads=[b_s, b_up], writes=[b_t])
                                P.op("pool", lambda e, t_t=t_t, G_t=G_t, h_t=h_t, c=ei * 2 + fc: e.tensor_tensor(h_t[:, c, :], t_t[:], G_t[:], ALU.mult),
                                     reads=[b_t, b_G], writes=[b_h])
                        for t4 in range(4):
                            T = tg * 4 + t4
                            y_t, b_y = pY2[0]
                            for half in range(2):
                                for c in range(8):
                                    s_ = (grp * 4 + c // 2) % NS
                                    P.op("pe", lambda e, c=c, s_=s_, t4=t4, half=half, h_t=h_t, y_t=y_t: e.matmul(
                                        y_t[:, half * 512:(half + 1) * 512], h_t[:, c, t4 * 128:(t4 + 1) * 128],
                                        wd[s_][0][:, c % 2, half * 512:(half + 1) * 512], start=(c == 0), stop=(c == 7)),
                                        reads=[b_h, wd[s_][1]], writes=[b_y], inc=(c == 7 and half == 1))
                            P.op("dve", lambda e, T=T, y_t=y_t: e.tensor_tensor(xres[:, T, :], xres[:, T, :], y_t[:], ALU.add),
                                 reads=[b_y, b_xr[T]], writes=[b_xr[T]])
                    for e_ in range(NS + grp * 4, min(16, NS + grp * 4 + 4)):
                        load_expert(e_)
            if stopB == "B3":
                P.dma("sp", dbg["x"], xres[:], reads=b_xr, writes=[b_out])
                return
            if l == 1:
                for T in range(16):
                    P.dma("sp", out[T * 128:(T + 1) * 128, :], xres[:, T, :], reads=[b_xr[T]], writes=[b_out])
                return
            with Phase(P, f"B4{l}") as ph:
                Abc, b_Abc = ph.sb("Abc", [128, D], F32)
                Sbc, b_Sbc = ph.sb("Sbc", [128, D], F32)
                tmod, b_tmod = ph.sb("tmod", [128, D], F32)
                junk, b_junk = ph.sb("junk", [128, D], BF16)
                hmb = [ph.sb("hmb", [128, D], BF16) for _ in range(2)]
                hT = [ph.sb("hT", [128, 8, 128], BF16) for _ in range(2)]
                identb, b_identb = ph.sb("identb", [128, 128], BF16)
                ssx, b_ssx = ph.sb("ssx", [128, 16], F32)
                rsx, b_rsx = ph.sb("rsx", [128, 16], F32)
                pX = [ph.ps("pX", [128, 8, 128], BF16) for _ in range(2)]
                P.op("pool", lambda e: e.memset(identb[:], 1.0), writes=[b_identb])
                P.op("pool", lambda e: e.affine_select(identb[:], identb[:], [[-1, 128]], ALU.is_equal, 0.0,
                                                       base=0, channel_multiplier=1), reads=[b_identb], writes=[b_identb])
                P.op("dve", lambda e: e.memset(ssx[:], 0.0), writes=[b_ssx])
                P.dma("sp", Sbc[:], mr[1:2, 0:D].to_broadcast([128, D]), reads=[b_modrow], writes=[b_Sbc])
                P.dma("sp", Abc[:], mr[1:2, D:2 * D].to_broadcast([128, D]), reads=[b_modrow], writes=[b_Abc])
                P.dma("sp", tmod[:], norm_mix[1:2, :].to_broadcast([128, D]), writes=[b_tmod])
                P.op("dve", lambda e: e.scalar_tensor_tensor(Abc[:], Abc[:], 1.0, tmod[:], ALU.add, ALU.mult),
                     reads=[b_Abc, b_tmod], writes=[b_Abc])
                def b4_pre(T):
                    P.dma("pool", xmid.ap()[T * 128:(T + 1) * 128, :], xres[:, T, :], reads=[b_xr[T]], writes=[b_xmid])
                    m_t, b_m = hmb[T % 2]
                    P.op("act", lambda e, T=T: e.activation(junk[:], xres[:, T, :], AF.Square, accum_out=ssx[:, T:T + 1]),
                         reads=[b_xr[T]], writes=[b_junk, b_ssx])
                    P.op("act", lambda e, T=T: e.activation(rsx[:, T:T + 1], ssx[:, T:T + 1], AF.Ln, bias=EPS, scale=1.0 / D),
                         reads=[b_ssx], writes=[b_rsx])
                    P.op("act", lambda e, T=T: e.activation(rsx[:, T:T + 1], rsx[:, T:T + 1], AF.Exp, scale=-0.5), reads=[b_rsx], writes=[b_rsx])
                    P.op("dve", lambda e, T=T: e.scalar_tensor_tensor(tmod[:], xres[:, T, :], rsx[:, T:T + 1], Abc[:], ALU.mult, ALU.mult),
                         reads=[b_xr[T], b_rsx, b_Abc], writes=[b_tmod])
                    P.op("pool", lambda e, m_t=m_t: e.tensor_tensor(m_t[:], tmod[:], Sbc[:], ALU.add),
                         reads=[b_tmod, b_Sbc], writes=[b_m])

                def b4_pe(T):
                    m_t, b_m = hmb[T % 2]
                    x_t, b_xT = pX[T % 2]
                    for kc in range(8):
                        P.op("pe", lambda e, kc=kc, m_t=m_t, x_t=x_t: e.transpose(x_t[:, kc, :], m_t[:, kc * 128:(kc + 1) * 128], identb[:]),
                             reads=[b_m, b_identb], writes=[b_xT], inc=(kc == 7))

                def b4_back(T):
                    x_t, b_xT = pX[T % 2]
                    o_t, b_o = hT[T % 2]
                    P.op("act", lambda e, x_t=x_t, o_t=o_t: e.activation(o_t[:], x_t[:], AF.Copy), reads=[b_xT], writes=[b_o])
                    for c in range(4):
                        P.dma("sp", sendH[c].ap()[:, T * 128:(T + 1) * 128].rearrange("(k p) t -> p k t", p=128), o_t[:, 2 * c:2 * c + 2, :],
                              reads=[b_o], writes=[b_sendH[c]])

                b4_pre(0)
                b4_pe(0)
                for T in range(16):
                    if T + 1 < 16:
                        b4_pre(T + 1)
                    b4_back(T)
                    if T + 1 < 16:
                        b4_pe(T + 1)

    if stop == "X1":
        dbg["rA"] = dout("dbg_rA", [1024, S], BF16)
        for c in range(4):
            P.collective("AllGather", sendA[c], recvA[c], reads=[b_sendA[c]], writes=[b_recvA[c]])
            for rk in range(2):
                P.dma("sp", dbg["rA"][rk * 512 + c * 128: rk * 512 + (c + 1) * 128, :], recvA[c].ap()[rk * 128:(rk + 1) * 128, :],
                      reads=[b_recvA[c]], writes=[b_out])
        P.barrier()
        P.flush()
        P.stack.close()
        return nc
    if stop in ("B1", "B2", "B3", "B"):
        if stop in ("B1", "B3", "B"):
            dbg["x"] = dout("dbg_x", [128, 16, D])
        if stop == "B2":
            dbg["hfT"] = dout("dbg_hfT", [128, 8, OWN], BF16)
            dbg["gT"] = dout("dbg_gT", [16, OWN], BF16)
            dbg["lg"] = dout("dbg_lg", [128, 16, 20])
        if stop == "B":
            dbg["hT"] = dout("dbg_hT", [1024, OWN], BF16)
    phase_B(0, sendA, b_sendA, recvA, b_recvA, a_w_out, stopB=(stop if stop in ("B1", "B2", "B3") else None))
    if stop in ("B1", "B2", "B3", "B"):
        if stop == "B":
            P.dma("sp", dbg["x"], xmid.ap().rearrange("(t p) d -> p t d", p=128), reads=[b_xmid], writes=[b_out])
            for c in range(4):
                P.dma("sp", dbg["hT"][c * 256:(c + 1) * 256, :], sendH[c].ap(), reads=[b_sendH[c]], writes=[b_out])
        P.barrier()
        P.flush()
        P.stack.close()
        return nc

    with Phase(P, "C") as ph:
        KT1, _ = ph.sb("KT1", [128, 4, S], BF16)
        b_KT = [P.buf(f"KT{q}") for q in range(8)]
        Vp1, _ = ph.sb("Vp1", [128, 32, 8, 128], BF16)
        b_Vp = [P.buf(f"Vp{q}") for q in range(8)]
        cumK, _ = ph.sb("cumK", [128, 32, 8], F32)
        b_cum = [P.buf(f"cum{q}") for q in range(8)]
        QT1 = [ph.sb("QT1", [128, 4, 512], BF16) for _ in range(2)]
        QT1b = [ph.sb("QT1b", [128, 4, 512], BF16) for _ in range(2)]
        hmc = [ph.sb("hmc", [128, 8, 512], BF16) for _ in range(2)]
        wC, b_wC = ph.sb("wC", [128, 8, 1544], BF16)
        wstg = [ph.sb("wstg", [128, 1544], F32) for _ in range(2)]
        ident, b_ident = ph.sb("ident", [128, 128], BF16)
        tri, b_tri = ph.sb("tri", [128, 128], BF16)
        U, b_U = ph.sb("U", [128, 128], F32)
        onesf, b_onesf = ph.sb("onesf", [128, 128], F32)
        gq, b_gq_ = ph.sb("gq", [128, 1], F32)
        gk, b_gk_ = ph.sb("gk", [128, 1], F32)
        fbb, b_fbb = ph.sb("fbb", [128, 8], F32)
        sq, b_sq = ph.sb("sq", [128, 1024], F32)
        ssq, b_ssq = ph.sb("ssq", [128, 16], F32)
        rsq, b_rsq = ph.sb("rsq", [128, 16], F32)
        qkn2 = [ph.sb("qkn", [128, 1024], BF16) for _ in range(2)]
        zf2 = [ph.sb("zf", [128, 8], F32) for _ in range(2)]
        lfn2 = [ph.sb("lfn", [128, 8], F32) for _ in range(2)]
        carry, b_carry = ph.sb("carry", [1, 8], F32)
        cref, b_cref = ph.sb("cref", [1, 8], F32)
        biasq = [ph.sb("biasq", [128, 32, 8], F32) for _ in range(2)]
        PT = [ph.sb("PT", [128, 512], BF16) for _ in range(3)]
        rec, b_rec = ph.sb("rec", [128, 512], F32)
        oTc = [ph.sb("oTc", [64, 512], BF16) for _ in range(2)]
        pA, b_pA = ph.ps("pA", [128, 1024])
        pT, b_pT = ph.ps("pT", [128, 8, 128], BF16)
        pS = [ph.ps("pS", [128, 512]) for _ in range(3)]
        pO, b_pO = ph.ps("pO", [128, 512])
        pB, b_pB = pA[:, 0:512], b_pA
        pMi, b_pMi = ph.ps("pMi", [128, 512])

        P.op("pool", lambda e: e.memset(ident[:], 1.0), writes=[b_ident])
        P.op("pool", lambda e: e.affine_select(ident[:], ident[:], [[-1, 128]], ALU.is_equal, 0.0,
                                               base=0, channel_multiplier=1), reads=[b_ident], writes=[b_ident])
        P.op("pool", lambda e: e.memset(tri[:], 1.0), writes=[b_tri])
        P.op("pool", lambda e: e.affine_select(tri[:], tri[:], [[1, 128]], ALU.is_ge, 0.0,
                                               base=0, channel_multiplier=-1), reads=[b_tri], writes=[b_tri])
        P.op("pool", lambda e: e.memset(U[:], 1.0), writes=[b_U])
        P.op("pool", lambda e: e.affine_select(U[:], U[:], [[1, 128]], ALU.is_ge, 0.0,
                                               base=0, channel_multiplier=-1), reads=[b_U], writes=[b_U])
        P.op("pool", lambda e: e.memset(onesf[:], 1.0), writes=[b_onesf])
        P.op("dve", lambda e: e.memset(Vp1[:], 1.0), writes=b_Vp)
        P.op("dve", lambda e: e.memset(carry[:], 0.0), writes=[b_carry])
        for i in range(2):
            P.op("dve", lambda e, i=i: e.memset(QT1[i][0][:], 0.0), writes=[QT1[i][1]])
            P.op("dve", lambda e, i=i: e.memset(QT1b[i][0][:], 0.0), writes=[QT1[i][1]])
        P.dma("sp", gq[:], b_gq, writes=[b_gq_])
        P.dma("sp", gk[:], b_gk, writes=[b_gk_])
        P.dma("sp", fbb[:], b_fb[0:1, :].to_broadcast([128, 8]), writes=[b_fbb])
        for kc in range(8):
            s_t, b_s = wstg[kc % 2]
            P.dma("sp", s_t[:], b_w_in[kc * 128:(kc + 1) * 128, :], writes=[b_s])
            P.op("dve", lambda e, s_t=s_t, kc=kc: e.tensor_copy(wC[:, kc, :], s_t[:]), reads=[b_s], writes=[b_wC])
        for c in range(4):
            P.collective("AllGather", sendH[c], recvH[c], reads=[b_sendH[c]], writes=[b_recvH[c]])

        def project(qg):
            h_t, b_h = hmc[qg % 2]
            q_t, b_q = QT1[qg % 2]
            rank, col0 = qg // 4, (qg % 4) * 512
            for kc in range(8):
                c = kc // 2
                r0 = rank * 256 + (kc % 2) * 128
                P.dma("sp", h_t[:, kc, :], recvH[c].ap()[r0:r0 + 128, col0:col0 + 512], reads=[b_recvH[c]], writes=[b_h])

            def st_qk(tb4):
                tsl = slice(tb4 * 128, (tb4 + 1) * 128)
                qn_t, b_qn = qkn2[tb4 % 2]
                for kc in range(8):
                    for half in range(2):
                        P.op("pe", lambda e, kc=kc, half=half, tsl=tsl: e.matmul(
                            pA[:, half * 512:(half + 1) * 512], h_t[:, kc, tsl], wC[:, kc, half * 512:(half + 1) * 512],
                            start=(kc == 0), stop=(kc == 7)),
                            reads=[b_h, b_wC], writes=[b_pA], inc=(kc == 7 and half == 1))
                P.op("act", lambda e: e.activation(sq[:], pA[:], AF.Square), reads=[b_pA], writes=[b_sq])
                P.op("dve", lambda e: e.tensor_reduce(ssq[:], sq[:].rearrange("p (h d) -> p h d", d=64), AX.X, ALU.add),
                     reads=[b_sq], writes=[b_ssq])
                P.op("act", lambda e: e.activation(rsq[:], ssq[:], AF.Ln, bias=EPS, scale=1.0 / 64), reads=[b_ssq], writes=[b_rsq])
                P.op("act", lambda e: e.activation(rsq[:], rsq[:], AF.Exp, scale=-0.5), reads=[b_rsq], writes=[b_rsq])
                P.op("dve", lambda e, qn_t=qn_t: e.tensor_tensor(qn_t[:].rearrange("p (h d) -> p h d", d=64),
                                                      pA[:].rearrange("p (h d) -> p h d", d=64),
                                                      rsq[:].unsqueeze(2).to_broadcast([128, 16, 64]), ALU.mult),
                     reads=[b_pA, b_rsq], writes=[b_qn])

            def st_vf(tb4):
                tb = 4 * qg + tb4
                tsl = slice(tb4 * 128, (tb4 + 1) * 128)
                z_t, b_z = zf2[tb4 % 2]
                l_t, b_l = lfn2[tb4 % 2]
                pv_t, b_pv = pS[0]
                pf_t, b_pf = pS[1]
                for kc in range(8):
                    P.op("pe", lambda e, kc=kc, tsl=tsl: e.matmul(pv_t[:, 0:512], h_t[:, kc, tsl], wC[:, kc, 1024:1536],
                                                                 start=(kc == 0), stop=(kc == 7)),
                         reads=[b_h, b_wC], writes=[b_pv], inc=False)
                    P.op("pe", lambda e, kc=kc, tsl=tsl: e.matmul(pf_t[:, 0:8], h_t[:, kc, tsl], wC[:, kc, 1536:1544],
                                                                 start=(kc == 0), stop=(kc == 7)),
                         reads=[b_h, b_wC], writes=[b_pf], inc=(kc == 7))
                P.op("act", lambda e, tb=tb: e.activation(Vp1[:, tb, :, 0:64], pv_t[:, 0:512].rearrange("p (h d) -> p h d", d=64), AF.Copy),
                     reads=[b_pv], writes=[b_Vp[qg]])
                P.op("dve", lambda e, z_t=z_t: e.tensor_tensor(z_t[:], pf_t[:, 0:8], fbb[:], ALU.add), reads=[b_pf, b_fbb], writes=[b_z])
                P.op("act", lambda e, z_t=z_t: e.activation(z_t[:], z_t[:], AF.Exp, scale=-1.0), reads=[b_z], writes=[b_z])
                P.op("act", lambda e, z_t=z_t: e.activation(z_t[:], z_t[:], AF.Ln, bias=1.0), reads=[b_z], writes=[b_z])
                P.op("dve", lambda e, z_t=z_t, l_t=l_t: e.tensor_scalar(l_t[:], z_t[:], -1.0, None, ALU.mult), reads=[b_z], writes=[b_l])

            def st_cs(tb4):
                tb = 4 * qg + tb4
                l_t, b_l = lfn2[tb4 % 2]
                P.op("pe", lambda e, l_t=l_t: e.matmul(pMi[:, 0:8], U[:], l_t[:], start=True, stop=False),
                     reads=[b_U, b_l], writes=[b_pMi], inc=False)
                P.op("pe", lambda e: e.matmul(pMi[:, 0:8], onesf[0:1, :], carry[0:1, :], start=False, stop=True),
                     reads=[b_onesf, b_carry], writes=[b_pMi], inc=False)
                P.op("pe", lambda e, l_t=l_t: e.matmul(pMi[0:1, 8:16], onesf[:, 0:1], l_t[:], start=True, stop=False),
                     reads=[b_onesf, b_l], writes=[b_pMi], inc=False)
                P.op("pe", lambda e: e.matmul(pMi[0:1, 8:16], onesf[0:1, 0:1], carry[0:1, :], start=False, stop=True),
                     reads=[b_onesf, b_carry], writes=[b_pMi], inc=True)
                P.op("dve", lambda e, tb=tb: e.tensor_copy(cumK[:, tb, :], pMi[:, 0:8]), reads=[b_pMi], writes=[b_cum[qg]])
                P.op("dve", lambda e: e.tensor_copy(carry[:], pMi[0:1, 8:16]), reads=[b_pMi], writes=[b_carry])
                if tb4 == 1:
                    P.op("dve", lambda e: e.tensor_copy(cref[:], pMi[0:1, 8:16]), reads=[b_pMi], writes=[b_cref])

            def st_tr(tb4):
                tb = 4 * qg + tb4
                tsl = slice(tb4 * 128, (tb4 + 1) * 128)
                qn_t, b_qn = qkn2[tb4 % 2]
                for c8 in range(8):
                    P.op("pe", lambda e, c8=c8, qn_t=qn_t: e.transpose(pT[:, c8, :], qn_t[:, c8 * 128:(c8 + 1) * 128], ident[:]),
                         reads=[b_qn, b_ident], writes=[b_pT], inc=(c8 == 7))
                qb_t = QT1b[qg % 2][0]
                P.op("act", lambda e, tsl=tsl: e.activation(q_t[0:64, :, tsl], pT[0:64, 0:4, :], AF.Copy, scale=gq[0:64, 0:1]),
                     reads=[b_pT, b_gq_], writes=[b_q])
                P.op("act", lambda e, qb_t=qb_t, tsl=tsl: e.activation(qb_t[64:128, :, tsl], pT[64:128, 0:4, :], AF.Copy, scale=gq[64:128, 0:1]),
                     reads=[b_pT, b_gq_], writes=[b_q])
                P.op("dve", lambda e, tb=tb: e.tensor_scalar(KT1[:, :, tb * 128:(tb + 1) * 128], pT[:, 4:8, :], gk[:, 0:1], None, ALU.mult),
                     reads=[b_pT, b_gk_], writes=[b_KT[qg]])

            st_qk(0)
            st_vf(0)
            for tb4 in range(4):
                if tb4 + 1 < 4:
                    st_qk(tb4 + 1)
                    st_vf(tb4 + 1)
                st_cs(tb4)
                st_tr(tb4)
            bq_t, b_bq = biasq[qg % 2]
            nkb = 4 * qg + 4
            P.op("pe", lambda e: e.matmul(pMi[:, 16:24], onesf[0:1, :], cref[0:1, :], start=True, stop=True),
                 reads=[b_onesf, b_cref], writes=[b_pMi])
            P.op("dve", lambda e, bq_t=bq_t, nkb=nkb: e.tensor_tensor(bq_t[:, 0:nkb, :], pMi[:, 16:24].unsqueeze(1).to_broadcast([128, nkb, 8]),
                                                                      cumK[:, 0:nkb, :], ALU.subtract),
                 reads=[b_pMi] + b_cum[0:qg + 1], writes=[b_bq])

        cntS = [0]
        ko = [0]

        def attend(qg):
            q_t, b_q = QT1[qg % 2]
            bq_t, b_bq = biasq[qg % 2]
            nkb = 4 * qg + 4
            for h in range(8):
                pr, hp = h // 2, h % 2
                lo, hi = 64 * hp, 64 * hp + 64
                items = []
                for kb in range(nkb):
                    i = kb - 4 * qg
                    c0 = 128 * max(i, 0)
                    items.append((kb, i, c0))

                def s_mm(idx):
                    kb, i, c0 = items[idx]
                    s_t, b_s = pS[(cntS[0] + idx) % 3]
                    qz = q_t if hp == 0 else QT1b[qg % 2][0]
                    P.op("pe", lambda e, kb=kb, c0=c0, s_t=s_t, pr=pr, qz=qz: e.matmul(s_t[:, c0:512], KT1[:, pr, kb * 128:(kb + 1) * 128],
                                                                       qz[:, pr, c0:512], start=True, stop=True),
                         reads=[b_KT[kb // 4], b_q], writes=[b_s])

                s_mm(0)
                if len(items) > 1:
                    s_mm(1)
                for idx, (kb, i, c0) in enumerate(items):
                    if idx + 2 < len(items):
                        s_mm(idx + 2)
                    s_t, b_s = pS[(cntS[0] + idx) % 3]
                    p_t, b_p = PT[(cntS[0] + idx) % 3]
                    P.op("act", lambda e, kb=kb, c0=c0, s_t=s_t, p_t=p_t, h=h: e.activation(p_t[:, c0:512], s_t[:, c0:512], AF.Exp,
                                                                                      bias=bq_t[:, kb, h:h + 1], scale=0.125),
                         reads=[b_s, b_bq], writes=[b_p])
                    if i >= 0:
                        P.op("pool", lambda e, c0=c0, p_t=p_t: e.tensor_tensor(p_t[:, c0:c0 + 128], p_t[:, c0:c0 + 128], tri[:], ALU.mult),
                             reads=[b_p, b_tri], writes=[b_p])
                    P.op("pe", lambda e, kb=kb, c0=c0, p_t=p_t, idx=idx, h=h, n_=len(items): e.matmul(pO[:, c0:512], Vp1[:, kb, h, :], p_t[:, c0:512],
                                                                                start=(idx == 0), stop=(idx == n_ - 1)),
                         reads=[b_Vp[kb // 4], b_p], writes=[b_pO], inc=(idx == len(items) - 1))
                cntS[0] += len(items)
                o_t, b_o = oTc[ko[0] % 2]
                ko[0] += 1
                P.op("dve", lambda e: e.reciprocal(rec[64:65, :], pO[64:65, :]), reads=[b_pO], writes=[b_rec])
                P.op("pe", lambda e: e.matmul(pB[0:64, :], onesf[64:65, 0:64], rec[64:65, :], start=True, stop=True),
                     reads=[b_onesf, b_rec], writes=[b_pB])
                P.op("act", lambda e: e.activation(rec[0:64, :], pB[0:64, :], AF.Copy), reads=[b_pB], writes=[b_rec])
                P.op("dve", lambda e, o_t=o_t: e.tensor_tensor(o_t[:], pO[0:64, :], rec[0:64, :], ALU.mult),
                     reads=[b_pO, b_rec], writes=[b_o])
                row = (h % 2) * 64
                P.dma("sp", sendC[h // 2].ap()[row:row + 64, qg * 512:(qg + 1) * 512], o_t[:], reads=[b_o], writes=[b_sendC[h // 2]])

        project(0)
        for qg in range(8):
            if qg + 1 < 8:
                project(qg + 1)
            attend(qg)

    if stop == "C":
        dbg["oC"] = dout("dbg_oC", [512, S], BF16)
        for c in range(4):
            P.dma("sp", dbg["oC"][c * 128:(c + 1) * 128, :], sendC[c].ap(), reads=[b_sendC[c]], writes=[b_out])
        P.barrier()
        P.flush()
        P.stack.close()
        return nc

    phase_B(1, sendC, b_sendC, recvC, b_recvC, b_w_out)
    P.barrier()
    P.flush()
    P.stack.close()
    return nc


def make_in_maps(inp, small=False):
    f = lambda a: np.ascontiguousarray(np.asarray(a, dtype=np.float32))
    x = f(inp["x"]); c = f(inp["c"])
    a_w_in = f(inp["a_w_in"])[0].reshape(D, 3, 3, 16, 64)
    b_w_in = f(inp["b_w_in"])[0]
    rel = f(inp["rel_bias"]).reshape(32, 3, 16)
    oh = t5_onehot()
    r_w = f(np.concatenate([inp["router_group_w"], inp["router_expert_w"]], axis=2))
    r_b = f(np.concatenate([inp["router_group_b"], inp["router_expert_b"]], axis=1))
    maps = []
    for core in range(8):
        b, hh = core // 2, core % 2
        hs = slice(hh * 8, hh * 8 + 8)
        awi = np.empty((D, 2, 3, 768), np.float32)
        for p in range(2):
            for g in range(3):
                for s_ in range(3):
                    awi[:, p, g, s_ * 256:(s_ + 1) * 256] = a_w_in[:, g, s_, hh * 8 + 4 * p: hh * 8 + 4 * p + 4, :].reshape(D, 256)
        bwi = np.concatenate([b_w_in[:, s_ * 1024 + hh * 512: s_ * 1024 + hh * 512 + 512] for s_ in range(3)]
                             + [b_w_in[:, 3072 + hh * 8: 3072 + hh * 8 + 8]], axis=1)
        selv = np.zeros((128, 2), np.float32); selv[:, hh] = 1.0
        m = dict(
            x_b=x[b], x_own=f(x[b, hh * OWN:(hh + 1) * OWN]),
            c_col=f(c[b].reshape(8, 128).T),
            w_ada=f(inp["w_ada"]), b_ada=f(inp["b_ada"]),
            norm_mix=f(inp["norm_mix"]), norm_ffn=f(inp["norm_ffn"]),
            relb=f(rel[:, :, hs]), ohc=oh,
            a_w_in=awi, a_w_out=f(inp["a_w_out"])[0],
            a_gq=f(np.tile(f(inp["a_q_norm"])[0], 2)[:, None]), a_gk=f(np.tile(f(inp["a_k_norm"])[0], 2)[:, None]),
            b_w_in=f(bwi), b_fb=f(f(inp["b_f_bias"])[:, hs]), b_w_out=f(inp["b_w_out"])[0],
            b_gq=f(np.tile(f(inp["b_q_norm"])[0], 2)[:, None]), b_gk=f(np.tile(f(inp["b_k_norm"])[0], 2)[:, None]),
            r_w=r_w, r_b=r_b,
            w_gate=f(inp["w_gate"]) if not small else np.zeros((1, 1, 8, 8), np.float32),
            w_up=f(inp["w_up"]) if not small else np.zeros((1, 1, 8, 8), np.float32),
            w_down=f(inp["w_down"]) if not small else np.zeros((1, 1, 8, 8), np.float32),
            sel=selv,
        )
        maps.append(m)
    return maps


_NC_CACHE = {}


def kernel(**inputs):
    maps = make_in_maps(inputs)
    if "nc" not in _NC_CACHE:
        _NC_CACHE["nc"] = build()
    res = run_bass_kernel_spmd(_NC_CACHE["nc"], maps, core_ids=list(range(8)))
    outp = np.empty((4, S, D), np.float32)
    for core in range(8):
        b, hh = core // 2, core % 2
        outp[b, hh * OWN:(hh + 1) * OWN] = res.results[core]["out"]
    return outp
```
